# Optimizing a Trainium2 kernel written in Bass

```python
import jax
import jax.numpy as jnp
from jax import lax
import numpy as np

D_MODEL = 1024
BATCH = 16
SEQ = 2048
DEPTH = 2

GRID_W = 64
CTX_LEN = 256
N_MIXERS = 4
GROUP_WIDTH = D_MODEL // N_MIXERS
MIX_WIDTH = N_MIXERS * GROUP_WIDTH
HEAD_DIM = 64
HGRN_HEADS = GROUP_WIDTH // HEAD_DIM
HGRN_CHUNK = 16
CONV_WIDTH = 3
RET_HEADS = GROUP_WIDTH // HEAD_DIM
RET_CHUNK = 128
ATT_Q_HEADS = GROUP_WIDTH // HEAD_DIM
ATT_KV_HEADS = 2
ATT_GROUP = ATT_Q_HEADS // ATT_KV_HEADS
KV_WIDTH = ATT_KV_HEADS * HEAD_DIM
ATT_BLOCK_Q = 128
ROPE_THETA = 10000.0
N_EXPERT_GROUPS = 4
EXPERTS_PER_GROUP = 4
N_EXPERTS = N_EXPERT_GROUPS * EXPERTS_PER_GROUP
TOP_K_IN_GROUP = 2
D_EXPERT = D_MODEL // 2
NORM_EPS = 1e-6
SPLIT_SIZES = (GROUP_WIDTH,) * 12 + (GROUP_WIDTH, KV_WIDTH, KV_WIDTH)
IN_WIDTH = 13 * GROUP_WIDTH + 2 * KV_WIDTH

kernel_name = 'hybrid_head_group_diffusion_block'


def rms_norm(x, gain):
    xf = x.astype(jnp.float32)
    y = xf * lax.rsqrt(jnp.mean(xf * xf, axis=-1, keepdims=True) + NORM_EPS)
    return (y * gain.astype(jnp.float32)).astype(x.dtype)


def head_rms_norm(a, gain):
    b, t, w = a.shape
    af = a.astype(jnp.float32).reshape(b, t, w // HEAD_DIM, HEAD_DIM)
    af = af * lax.rsqrt(jnp.mean(af * af, axis=-1, keepdims=True) + NORM_EPS)
    return (af.reshape(b, t, w) * gain.astype(jnp.float32)).astype(a.dtype)


def modulate(h, shift, scale):
    return h * (1.0 + scale) + shift


def split_heads(a, n_heads):
    b, t, _ = a.shape
    return a.reshape(b, t, n_heads, -1).transpose(0, 2, 1, 3)


def merge_heads(a):
    b, h, t, d = a.shape
    return a.transpose(0, 2, 1, 3).reshape(b, t, h * d)


def split_columns(p):
    parts, start = [], 0
    for width in SPLIT_SIZES:
        parts.append(p[..., start:start + width])
        start += width
    return parts


def flip_time(a, direction):
    return a if direction == 0 else jnp.flip(a, axis=2)


def axial_rope_tables(rows):
    t = jnp.arange(rows * GRID_W)
    row = (t // GRID_W).astype(jnp.float32)
    col = (t % GRID_W).astype(jnp.float32)
    n_freq = HEAD_DIM // 4
    inv_freq = ROPE_THETA ** (-jnp.arange(n_freq, dtype=jnp.float32) / n_freq)
    ang = jnp.concatenate([row[:, None] * inv_freq, col[:, None] * inv_freq], axis=-1)
    return jnp.cos(ang), jnp.sin(ang)


def apply_rope(a, cos, sin):
    af = a.astype(jnp.float32).reshape(a.shape[:-1] + (HEAD_DIM // 2, 2))
    x1, x2 = af[..., 0], af[..., 1]
    out = jnp.stack([x1 * cos - x2 * sin, x1 * sin + x2 * cos], axis=-1)
    return out.reshape(a.shape).astype(a.dtype)


def gla_chunk(q, k, v, log_f, s0):
    out_dtype = v.dtype
    q, k, v, log_f = (a.astype(jnp.float32) for a in (q, k, v, log_f))
    b, h, t, dk = q.shape
    dv = v.shape[-1]
    c = HGRN_CHUNK
    n = t // c
    q, k, log_f = (a.reshape(b, h, n, c, dk) for a in (q, k, log_f))
    v = v.reshape(b, h, n, c, dv)
    cum = jnp.cumsum(log_f, axis=3)
    causal = jnp.tril(jnp.ones((c, c), dtype=bool))
    rel = cum[:, :, :, :, None, :] - cum[:, :, :, None, :, :]
    decay = jnp.exp(jnp.where(causal[:, :, None], rel, -jnp.inf))
    scores = jnp.einsum('bhntd,bhnsd,bhntsd->bhnts', q, k, decay)
    o_intra = jnp.einsum('bhnts,bhnse->bhnte', scores, v)
    last = cum[:, :, :, -1]
    chunk_kv = jnp.einsum('bhnsd,bhnse->bhnde', k * jnp.exp(last[:, :, :, None] - cum), v)

    def step(state, inp):
        kv_c, last_c = inp
        return state * jnp.exp(last_c)[..., None] + kv_c, state

    s_final, s_in = lax.scan(step, s0.astype(jnp.float32),
                             (jnp.moveaxis(chunk_kv, 2, 0), jnp.moveaxis(last, 2, 0)))
    s_in = jnp.moveaxis(s_in, 0, 2)
    o_inter = jnp.einsum('bhntd,bhnde->bhnte', q * jnp.exp(cum), s_in)
    return (o_intra + o_inter).reshape(b, h, t, dv).astype(out_dtype), s_final


def gla_final_state(k, v, log_f):
    k, v, log_f = (a.astype(jnp.float32) for a in (k, v, log_f))
    cum = jnp.cumsum(log_f, axis=2)
    return jnp.einsum('bhsd,bhse->bhde', k * jnp.exp(cum[:, :, -1:] - cum), v)


def retention_chunk(q, k, v, log_gamma, s0):
    out_dtype = v.dtype
    q, k, v = (a.astype(jnp.float32) for a in (q, k, v))
    b, h, t, dk = q.shape
    dv = v.shape[-1]
    c = RET_CHUNK
    n = t // c
    q, k = (a.reshape(b, h, n, c, dk) for a in (q, k))
    v = v.reshape(b, h, n, c, dv)
    lg = log_gamma.astype(jnp.float32)[:, None]
    pos = jnp.arange(c, dtype=jnp.float32)
    rel = pos[:, None] - pos[None, :]
    decay = jnp.where(rel >= 0, jnp.exp(lg[:, :, None] * jnp.maximum(rel, 0.0)), 0.0)
    scores = jnp.einsum('bhntd,bhnsd->bhnts', q, k) * decay[None, :, None]
    o_intra = jnp.einsum('bhnts,bhnse->bhnte', scores, v)
    k_dec = k * jnp.exp(lg * (c - 1 - pos))[None, :, None, :, None]
    chunk_kv = jnp.einsum('bhnsd,bhnse->bhnde', k_dec, v)
    chunk_decay = jnp.exp(lg[:, 0] * c)[None, :, None, None]

    def step(state, kv_c):
        return state * chunk_decay + kv_c, state

    s_final, s_in = lax.scan(step, s0.astype(jnp.float32), jnp.moveaxis(chunk_kv, 2, 0))
    s_in = jnp.moveaxis(s_in, 0, 2)
    o_inter = jnp.einsum('bhntd,bhnde->bhnte', q * jnp.exp(lg * (pos + 1.0))[None, :, None, :, None], s_in)
    return (o_intra + o_inter).reshape(b, h, t, dv).astype(out_dtype), s_final


def retention_final_state(k, v, log_gamma):
    k, v = k.astype(jnp.float32), v.astype(jnp.float32)
    length = k.shape[2]
    lg = log_gamma.astype(jnp.float32)[:, None]
    w = jnp.exp(lg * (length - 1 - jnp.arange(length, dtype=jnp.float32)))
    return jnp.einsum('bhsd,bhse->bhde', k * w[None, :, :, None], v)


def hgrn2_mixer(px, pc, lower_bound, norm_gain, with_ctx_out):
    def key_and_log_decay(z, direction):
        lb = lower_bound[direction]
        f = lb + (1.0 - lb) * jax.nn.sigmoid(z.astype(jnp.float32))
        return split_heads(1.0 - f, HGRN_HEADS), split_heads(jnp.log(f), HGRN_HEADS)

    qx, ix = split_heads(px[0], HGRN_HEADS), split_heads(px[1], HGRN_HEADS)
    ic = split_heads(pc[1], HGRN_HEADS)
    zero = jnp.zeros((ic.shape[0], HGRN_HEADS, HEAD_DIM, HEAD_DIM), jnp.float32)
    ox, oc = 0.0, 0.0
    for direction in range(2):
        kx, lfx = key_and_log_decay(px[2 + direction], direction)
        kc, lfc = key_and_log_decay(pc[2 + direction], direction)
        if with_ctx_out:
            qc = split_heads(pc[0], HGRN_HEADS)
            oc_d, s_ctx = gla_chunk(flip_time(qc, direction), flip_time(kc, direction),
                                    flip_time(ic, direction), flip_time(lfc, direction), zero)
            oc = oc + flip_time(oc_d, direction)
        else:
            s_ctx = gla_final_state(flip_time(kc, direction), flip_time(ic, direction), flip_time(lfc, direction))
        ox_d, _ = gla_chunk(flip_time(qx, direction), flip_time(kx, direction),
                            flip_time(ix, direction), flip_time(lfx, direction), s_ctx)
        ox = ox + flip_time(ox_d, direction)
    out_x = head_rms_norm(merge_heads(ox), norm_gain) * jax.nn.silu(px[4])
    out_c = head_rms_norm(merge_heads(oc), norm_gain) * jax.nn.silu(pc[4]) if with_ctx_out else None
    return out_x, out_c


def depthwise_conv_centred(u, w):
    return lax.conv_general_dilated(
        u, w[:, None, :].astype(u.dtype), window_strides=(1,),
        padding=[(CONV_WIDTH // 2, CONV_WIDTH // 2)],
        dimension_numbers=('NWC', 'WIO', 'NWC'), feature_group_count=u.shape[-1])


def short_conv_mixer(px, pc, conv_w, with_ctx_out):
    def run(p):
        b_gate, c_gate, h = p
        return b_gate * depthwise_conv_centred(c_gate * h, conv_w)
    return run(px), (run(pc) if with_ctx_out else None)


def retention_mixer(px, pc, log_gamma, norm_gain, with_ctx_out):
    scale = HEAD_DIM ** -0.5

    def qkv(p):
        return split_heads(p[0], RET_HEADS) * scale, split_heads(p[1], RET_HEADS), split_heads(p[2], RET_HEADS)

    qx, kx, vx = qkv(px)
    qc, kc, vc = qkv(pc)
    zero = jnp.zeros((kc.shape[0], RET_HEADS, HEAD_DIM, HEAD_DIM), jnp.float32)
    ox, oc = 0.0, 0.0
    for direction in range(2):
        if with_ctx_out:
            oc_d, s_ctx = retention_chunk(flip_time(qc, direction), flip_time(kc, direction),
                                          flip_time(vc, direction), log_gamma[direction], zero)
            oc = oc + flip_time(oc_d, direction)
        else:
            s_ctx = retention_final_state(flip_time(kc, direction), flip_time(vc, direction), log_gamma[direction])
        ox_d, _ = retention_chunk(flip_time(qx, direction), flip_time(kx, direction),
                                  flip_time(vx, direction), log_gamma[direction], s_ctx)
        ox = ox + flip_time(ox_d, direction)
    out_x = head_rms_norm(merge_heads(ox), norm_gain) * jax.nn.silu(px[3])
    out_c = head_rms_norm(merge_heads(oc), norm_gain) * jax.nn.silu(pc[3]) if with_ctx_out else None
    return out_x, out_c


def gqa_softmax(q, k, v):
    s = jnp.einsum('bkgqd,bknd->bkgqn', q, k, preferred_element_type=jnp.float32) * (HEAD_DIM ** -0.5)
    p = jax.nn.softmax(s, axis=-1)
    return jnp.einsum('bkgqn,bknd->bkgqd', p.astype(v.dtype), v)


def blocked_attention(q, k, v):
    b, hq, t, d = q.shape
    nb = t // ATT_BLOCK_Q
    qb = q.reshape(b, ATT_KV_HEADS, ATT_GROUP, nb, ATT_BLOCK_Q, d).transpose(3, 0, 1, 2, 4, 5)
    ob = lax.map(lambda qi: gqa_softmax(qi, k, v), qb)
    return ob.transpose(1, 2, 3, 0, 4, 5).reshape(b, hq, t, d)


def attention_mixer(px, pc, q_norm, k_norm, rope_cos, rope_sin, with_ctx_out):
    qx = apply_rope(rms_norm(split_heads(px[0], ATT_Q_HEADS), q_norm), rope_cos, rope_sin)
    kx = apply_rope(rms_norm(split_heads(px[1], ATT_KV_HEADS), k_norm), rope_cos, rope_sin)
    vx = split_heads(px[2], ATT_KV_HEADS)
    kc = rms_norm(split_heads(pc[1], ATT_KV_HEADS), k_norm)
    vc = split_heads(pc[2], ATT_KV_HEADS)
    keys = jnp.concatenate([kc, kx], axis=2)
    vals = jnp.concatenate([vc, vx], axis=2)
    out_x = merge_heads(blocked_attention(qx, keys, vals))
    out_c = None
    if with_ctx_out:
        qc = rms_norm(split_heads(pc[0], ATT_Q_HEADS), q_norm)
        b, _, length, d = qc.shape
        oc = gqa_softmax(qc.reshape(b, ATT_KV_HEADS, ATT_GROUP, length, d), kc, vc)
        out_c = merge_heads(oc.reshape(b, ATT_Q_HEADS, length, d))
    return out_x, out_c


def hierarchical_moe(h, w_rg, b_rg, w_re, b_re, w_gate, w_up, w_down):
    n = h.shape[0]
    group_prob = jax.nn.softmax((h @ w_rg + b_rg).astype(jnp.float32), axis=-1)
    group_p, group_idx = lax.top_k(group_prob, 1)
    expert_logits = (h @ w_re + b_re).astype(jnp.float32).reshape(n, N_EXPERT_GROUPS, EXPERTS_PER_GROUP)
    in_group = jnp.take_along_axis(expert_logits, group_idx[:, :, None], axis=1)[:, 0]
    expert_p, expert_idx = lax.top_k(jax.nn.softmax(in_group, axis=-1), TOP_K_IN_GROUP)
    expert_p = expert_p / jnp.sum(expert_p, axis=-1, keepdims=True)
    weights = group_p * expert_p
    expert_id = group_idx * EXPERTS_PER_GROUP + expert_idx
    combine = jnp.sum(jax.nn.one_hot(expert_id, N_EXPERTS, dtype=jnp.float32) * weights[..., None], axis=1)
    y = jnp.zeros(h.shape, jnp.float32)
    for e in range(N_EXPERTS):
        hidden = jax.nn.silu(h @ w_gate[e]) * (h @ w_up[e])
        y = y + combine[:, e:e + 1] * (hidden @ w_down[e]).astype(jnp.float32)
    return y.astype(h.dtype)


def setup_inputs(seed: int = 0) -> dict:
    key = jax.random.key(seed)
    ks = jax.random.split(key, 24)

    def nrm(k, shape, scale):
        return jax.random.normal(k, shape, jnp.float32) * scale

    ret_base_logit = jnp.log(2.0 ** (5.0 + jnp.arange(RET_HEADS, dtype=jnp.float32)) - 1.0)
    return {
        'x': nrm(ks[0], (BATCH, SEQ, D_MODEL), 1.0),
        'c': nrm(ks[1], (BATCH, D_MODEL), 1.0),
        'ctx': nrm(ks[2], (BATCH, CTX_LEN, D_MODEL), 1.0),
        'c_ctx': nrm(ks[3], (D_MODEL,), 1.0),
        'ada_w': nrm(ks[4], (DEPTH, D_MODEL, 6 * D_MODEL), 0.5 * D_MODEL ** -0.5),
        'ada_b': nrm(ks[5], (DEPTH, 6 * D_MODEL), 0.02),
        'norm_mix': 1.0 + nrm(ks[6], (DEPTH, D_MODEL), 0.02),
        'norm_ffn': 1.0 + nrm(ks[7], (DEPTH, D_MODEL), 0.02),
        'w_in': nrm(ks[8], (DEPTH, D_MODEL, IN_WIDTH), D_MODEL ** -0.5),
        'hgrn_lb_logits': nrm(ks[9], (DEPTH, 2, GROUP_WIDTH), 0.5),
        'hgrn_norm': 1.0 + nrm(ks[10], (DEPTH, GROUP_WIDTH), 0.02),
        'conv_w': nrm(ks[11], (DEPTH, CONV_WIDTH, GROUP_WIDTH), CONV_WIDTH ** -0.5),
        'ret_decay_logit': ret_base_logit + nrm(ks[12], (DEPTH, 2, RET_HEADS), 0.1),
        'ret_norm': 1.0 + nrm(ks[13], (DEPTH, GROUP_WIDTH), 0.02),
        'q_norm': 1.0 + nrm(ks[14], (DEPTH, HEAD_DIM), 0.02),
        'k_norm': 1.0 + nrm(ks[15], (DEPTH, HEAD_DIM), 0.02),
        'w_out': nrm(ks[16], (DEPTH, MIX_WIDTH, D_MODEL), MIX_WIDTH ** -0.5),
        'router_group_w': nrm(ks[17], (DEPTH, D_MODEL, N_EXPERT_GROUPS), D_MODEL ** -0.5),
        'router_group_b': nrm(ks[18], (DEPTH, N_EXPERT_GROUPS), 0.01),
        'router_expert_w': nrm(ks[19], (DEPTH, D_MODEL, N_EXPERTS), D_MODEL ** -0.5),
        'router_expert_b': nrm(ks[20], (DEPTH, N_EXPERTS), 0.01),
        'expert_w_gate': nrm(ks[21], (DEPTH, N_EXPERTS, D_MODEL, D_EXPERT), D_MODEL ** -0.5),
        'expert_w_up': nrm(ks[22], (DEPTH, N_EXPERTS, D_MODEL, D_EXPERT), D_MODEL ** -0.5),
        'expert_w_down': nrm(ks[23], (DEPTH, N_EXPERTS, D_EXPERT, D_MODEL), D_EXPERT ** -0.5),
    }


def reference(x, c, ctx, c_ctx, ada_w, ada_b, norm_mix, norm_ffn, w_in, hgrn_lb_logits, hgrn_norm,
              conv_w, ret_decay_logit, ret_norm, q_norm, k_norm, w_out, router_group_w, router_group_b,
              router_expert_w, router_expert_b, expert_w_gate, expert_w_up, expert_w_down):
    b, t, d = x.shape
    rows = t // GRID_W
    rope_cos, rope_sin = axial_rope_tables(rows)
    lb_w = jax.nn.softmax(hgrn_lb_logits.astype(jnp.float32), axis=0)
    lower_bounds = jnp.cumsum(lb_w, axis=0) - lb_w[0]
    silu_c = jax.nn.silu(c)
    silu_cc = jax.nn.silu(c_ctx)
    for layer in range(DEPTH):
        with_ctx_out = layer < DEPTH - 1
        mod_x = jnp.split(silu_c @ ada_w[layer] + ada_b[layer], 6, axis=-1)
        mod_c = jnp.split(silu_cc @ ada_w[layer] + ada_b[layer], 6, axis=-1)

        hx = modulate(rms_norm(x, norm_mix[layer]), mod_x[0][:, None], mod_x[1][:, None])
        hc = modulate(rms_norm(ctx, norm_mix[layer]), mod_c[0], mod_c[1])
        px = split_columns(hx @ w_in[layer])
        pc = split_columns(hc @ w_in[layer])
        log_gamma = jax.nn.log_sigmoid(ret_decay_logit[layer].astype(jnp.float32))
        ax, ac = hgrn2_mixer(px[0:5], pc[0:5], lower_bounds[layer], hgrn_norm[layer], with_ctx_out)
        bx, bc = short_conv_mixer(px[5:8], pc[5:8], conv_w[layer], with_ctx_out)
        rx, rc = retention_mixer(px[8:12], pc[8:12], log_gamma, ret_norm[layer], with_ctx_out)
        gx, gc = attention_mixer(px[12:15], pc[12:15], q_norm[layer], k_norm[layer], rope_cos, rope_sin,
                                 with_ctx_out)
        x = x + mod_x[2][:, None] * (jnp.concatenate([ax, bx, rx, gx], axis=-1) @ w_out[layer])

        hx2 = modulate(rms_norm(x, norm_ffn[layer]), mod_x[3][:, None], mod_x[4][:, None])
        moe_args = (router_group_w[layer], router_group_b[layer], router_expert_w[layer],
                    router_expert_b[layer], expert_w_gate[layer], expert_w_up[layer], expert_w_down[layer])
        if with_ctx_out:
            ctx = ctx + mod_c[2] * (jnp.concatenate([ac, bc, rc, gc], axis=-1) @ w_out[layer])
            hc2 = modulate(rms_norm(ctx, norm_ffn[layer]), mod_c[3], mod_c[4])
            y = hierarchical_moe(jnp.concatenate([hx2.reshape(-1, d), hc2.reshape(-1, d)], axis=0), *moe_args)
            x = x + mod_x[5][:, None] * y[:b * t].reshape(b, t, d)
            ctx = ctx + mod_c[5] * y[b * t:].reshape(ctx.shape)
        else:
            y = hierarchical_moe(hx2.reshape(-1, d), *moe_args)
            x = x + mod_x[5][:, None] * y.reshape(b, t, d)
    return x
```

```python
import numpy as np
import concourse.bass as bass
import concourse.mybir as mybir
from concourse.bass_utils import run_bass_kernel_spmd

F32 = mybir.dt.float32
BF16 = mybir.dt.bfloat16
AF = mybir.ActivationFunctionType
ALU = mybir.AluOpType

D = 1024
T = 2048
LC = 256
NT = LC + T
NTILE = NT // 128
DEPTH = 2
INW = 3584
EPS = 1e-6
ESZ = {F32: 4, BF16: 2}


class TInfo:
    def __init__(self, name, space, shape, dtype, handle, off, group):
        self.name, self.space, self.shape, self.dtype, self.h = name, space, tuple(shape), dtype, handle
        self.off = off
        self.group = group
        self.esz = ESZ[dtype]
        st = [1] * len(shape)
        for i in range(len(shape) - 2, 0, -1):
            st[i] = st[i + 1] * shape[i + 1]
        self.strides = st

    def __getitem__(self, idx):
        if not isinstance(idx, tuple):
            idx = (idx,)
        idx = idx + (slice(None),) * (len(self.shape) - len(idx))
        reg = []
        for i, s in zip(idx, self.shape):
            if isinstance(i, int):
                reg.append((i, i + 1))
            else:
                lo = 0 if i.start is None else i.start
                hi = s if i.stop is None else i.stop
                assert 0 <= lo < hi <= s, (self.name, idx)
                reg.append((lo, hi))
        return V(self.h[idx], self, tuple(reg))


class V:
    __slots__ = ("ap", "t", "reg", "blo", "bhi")

    def __init__(self, ap, t, reg):
        self.ap, self.t, self.reg = ap, t, reg
        lo = hi = 0
        for i in range(1, len(reg)):
            lo += reg[i][0] * t.strides[i]
            hi += (reg[i][1] - 1) * t.strides[i]
        self.blo = t.off + lo * t.esz
        self.bhi = t.off + (hi + 1) * t.esz

    def w(self, ap):
        return V(ap, self.t, self.reg)


def _overlap(a, b):
    if a.t.space == "ps":
        return True
    if a.reg[0][0] >= b.reg[0][1] or b.reg[0][0] >= a.reg[0][1]:
        return False
    if a.t is b.t:
        for (l1, h1), (l2, h2) in zip(a.reg[1:], b.reg[1:]):
            if l1 >= h2 or l2 >= h1:
                return False
        return True
    return a.blo < b.bhi and b.blo < a.bhi


def _covers(a, b):
    if a.t.space == "ps":
        return True
    if a.t is b.t:
        for (l1, h1), (l2, h2) in zip(a.reg, b.reg):
            if l1 > l2 or h1 < h2:
                return False
        return True
    return False


class Prog:
    CE = ("pe", "act", "dve", "pool")

    def __init__(self, nc, ndma=8):
        self.nc = nc
        self.ops = {e: [] for e in ("pe", "act", "dve", "pool", "sp")}
        self.cnt = {}
        self.known = {e: {} for e in self.ops}
        self.snap = {}
        self.groups = {}
        self.ndma = ndma
        self.dma_i = {"sp": 0, "pool": 0}
        self.tensors = {}
        self.nops = 0

    def sb(self, name, shape, dtype, off, group="sb"):
        h = self.nc.alloc_sbuf_tensor_at(name, list(shape), dtype, offset=off)
        t = TInfo(name, "sb", shape, dtype, h, off, group)
        nbytes = int(np.prod(shape[1:])) * t.esz
        t.end = (off + nbytes + 31) // 32 * 32
        self.tensors[name] = t
        return t

    def ps(self, name):
        h = self.nc.alloc_psum_tensor(name, [128, 512], F32)
        return TInfo(name, "ps", [128, 512], F32, h, 0, name)

    def dram(self, name, shape, dtype, kind):
        h = self.nc.dram_tensor(name, list(shape), dtype, kind=kind)
        return h

    def _deps(self, eng, reads, writes):
        need = {}

        def add(tok, weng):
            s, v = tok
            if need.get(s, 0) < v:
                need[s] = v

        for r in reads:
            g = self.groups.setdefault(r.t.group, ([], []))
            for (wv, tok, weng) in g[0]:
                if _overlap(wv, r):
                    add(tok, weng)
            if r.t.space == "ps":
                for (rv, tok, reng) in g[1]:
                    if reng != eng:
                        add(tok, reng)
        for w in writes:
            g = self.groups.setdefault(w.t.group, ([], []))
            for (wv, tok, weng) in g[0]:
                if _overlap(wv, w):
                    if not (tok[0] == eng and eng == "pe"):
                        add(tok, weng)
            for (rv, tok, reng) in g[1]:
                if _overlap(rv, w):
                    if not (tok[0] == eng and eng == "pe"):
                        add(tok, reng)
        return need

    def _commit(self, eng, reads, writes, tok):
        for r in reads:
            g = self.groups[r.t.group]
            lst = g[1]
            for i, (rv, t2, e2) in enumerate(lst):
                if t2[0] == tok[0] and rv.t is r.t and rv.reg == r.reg:
                    lst[i] = (r, tok, eng)
                    break
            else:
                lst.append((r, tok, eng))
        for w in writes:
            g = self.groups[w.t.group]
            g0 = [x for x in g[0] if not _covers(w, x[0])]
            g1 = [x for x in g[1] if not _covers(w, x[0])]
            g0.append((w, tok, eng))
            g[0][:] = g0
            g[1][:] = g1

    def _waits(self, eng, need):
        known = self.known[eng]
        waits = []
        for s, v in need.items():
            if known.get(s, 0) < v:
                waits.append((s, v))
        for s, v in waits:
            if known.get(s, 0) < v:
                known[s] = v
            sn = self.snap.get((s, v))
            if sn:
                for k2, v2 in sn.items():
                    if known.get(k2, 0) < v2:
                        known[k2] = v2
        return waits

    def op(self, eng, fn, reads=(), writes=()):
        reads = [r for r in reads if r is not None and r.t.space != "dram"]
        writes = [w for w in writes if w.t.space != "dram"]
        need = self._deps(eng, reads, writes)
        waits = self._waits(eng, need)
        n = self.cnt.get(eng, 0) + 1
        self.cnt[eng] = n
        tok = (eng, n)
        kn = self.known[eng]
        self.snap[tok] = dict(kn)
        self._commit(eng, reads, writes, tok)
        self.ops[eng].append((waits, fn, tok, 1))
        self.nops += 1
        return tok

    def dma(self, q, fn, reads=(), writes=()):
        reads = [r for r in reads if r.t.space != "dram"]
        writes = [w for w in writes if w.t.space != "dram"]
        need = self._deps("dma", reads, writes)
        i = self.dma_i[q]
        self.dma_i[q] = i + 1
        sem = "%s_d%d" % (q, i % self.ndma)
        prev = self.cnt.get(sem, 0)
        if prev:
            if need.get(sem, 0) < prev:
                need[sem] = prev
        waits = self._waits(q, need)
        self.cnt[sem] = prev + 16
        tok = (sem, prev + 16)
        self.snap[tok] = dict(self.known[q])
        self._commit("dma", reads, writes, tok)
        self.ops[q].append((waits, fn, tok, 16))
        self.nops += 1
        return tok

    def wait_all(self, eng, toks):
        need = {}
        for s, v in toks:
            if need.get(s, 0) < v:
                need[s] = v
        waits = self._waits(eng, need)
        if waits:
            self.ops[eng].append((waits, None, None, 0))

    def emit(self):
        nc = self.nc
        names = sorted(self.cnt.keys())
        sems = {}
        import contextlib
        with contextlib.ExitStack() as es:
            for s in names:
                sems[s] = es.enter_context(nc.semaphore("s_" + s))
            block = es.enter_context(nc.Block())

            def run(e, lst):
                for waits, fn, tok, amt in lst:
                    for s, v in waits:
                        e.wait_ge(sems[s], v)
                    if fn is not None:
                        ins = fn(e)
                        ins.then_inc(sems[tok[0]], amt)

            ops = self.ops

            @block.tensor
            def _(e):
                run(e, ops["pe"])

            @block.scalar
            def _(e):
                run(e, ops["act"])

            @block.vector
            def _(e):
                run(e, ops["dve"])

            @block.gpsimd
            def _(e):
                run(e, ops["pool"])

            @block.sync
            def _(e):
                run(e, ops["sp"])


class K:
    def __init__(self, P):
        self.P = P

    def mm(self, out, lhsT, rhs, start=True, stop=True):
        self.P.op("pe", lambda e: e.matmul(out.ap, lhsT.ap, rhs.ap, start=start, stop=stop),
                  reads=[lhsT, rhs], writes=[out])

    def transpose(self, out, in_, ident):
        self.P.op("pe", lambda e: e.transpose(out.ap, in_.ap, ident.ap), reads=[in_, ident], writes=[out])

    def act(self, out, in_, func, bias=None, scale=None, eng="act"):
        kw = {}
        rd = [in_]
        if bias is not None:
            if isinstance(bias, V):
                kw["bias"] = bias.ap
                rd.append(bias)
            else:
                kw["bias"] = bias
        if scale is not None:
            if isinstance(scale, V):
                kw["scale"] = scale.ap
                rd.append(scale)
            else:
                kw["scale"] = scale
        self.P.op("act", lambda e: e.activation(out.ap, in_.ap, func, **kw), reads=rd, writes=[out])

    def tt(self, out, in0, in1, op, eng="dve"):
        self.P.op(eng, lambda e: e.tensor_tensor(out.ap, in0.ap, in1.ap, op), reads=[in0, in1], writes=[out])

    def ts(self, out, in0, s1, op0, s2=None, op1=None, eng="dve"):
        rd = [in0]
        a1 = s1.ap if isinstance(s1, V) else s1
        a2 = s2.ap if isinstance(s2, V) else s2
        if isinstance(s1, V):
            rd.append(s1)
        if isinstance(s2, V):
            rd.append(s2)
        if op1 is None:
            self.P.op(eng, lambda e: e.tensor_scalar(out.ap, in0.ap, a1, None, op0), reads=rd, writes=[out])
        else:
            self.P.op(eng, lambda e: e.tensor_scalar(out.ap, in0.ap, a1, a2, op0, op1), reads=rd, writes=[out])

    def stt(self, out, in0, s, in1, op0, op1):
        rd = [in0, in1]
        a = s.ap if isinstance(s, V) else s
        if isinstance(s, V):
            rd.append(s)
        self.P.op("dve", lambda e: e.scalar_tensor_tensor(out.ap, in0.ap, a, in1.ap, op0, op1), reads=rd, writes=[out])

    def copy(self, out, in_, eng="dve"):
        if eng == "act":
            self.P.op("act", lambda e: e.copy(out.ap, in_.ap), reads=[in_], writes=[out])
        else:
            self.P.op(eng, lambda e: e.tensor_copy(out.ap, in_.ap), reads=[in_], writes=[out])

    def recip(self, out, in_):
        self.P.op("dve", lambda e: e.reciprocal(out.ap, in_.ap), reads=[in_], writes=[out])

    def memset(self, out, val, eng="dve"):
        self.P.op(eng, lambda e: e.memset(out.ap, val), writes=[out])

    def reduce(self, out, in_, op, eng="dve"):
        self.P.op(eng, lambda e: e.tensor_reduce(out.ap, in_.ap, mybir.AxisListType.X, op), reads=[in_], writes=[out])

    def load(self, out, src_ap, q="sp"):
        self.P.dma(q, lambda e: e.dma_start(out=out.ap, in_=src_ap), writes=[out])

    def store(self, dst_ap, in_, q="sp"):
        return self.P.dma(q, lambda e: e.dma_start(out=dst_ap, in_=in_.ap), reads=[in_])


class Pack:
    def __init__(self, rows):
        self.rows = rows
        self.cols = 0
        self.ent = {}
        self.parts = []

    def add(self, name, arr):
        arr = np.ascontiguousarray(arr, dtype=np.float32).reshape(self.rows, -1)
        self.ent[name] = (self.cols, arr.shape[1])
        self.cols += arr.shape[1]
        self.parts.append(arr)

    def array(self):
        return np.ascontiguousarray(np.concatenate(self.parts, axis=1))


def fm(v):
    v = np.asarray(v)
    return v.reshape(-1, 128).T


def hd(v):
    a = np.asarray(v).reshape(4, 64).T
    return np.concatenate([a, a], axis=0)


def pv_layout():
    p = Pack(128)
    z = np.zeros
    for l in range(DEPTH):
        p.add("nmix%d" % l, z((128, 8)))
        p.add("nffn%d" % l, z((128, 8)))
        p.add("adab%d" % l, z((128, 48)))
        p.add("qn%d" % l, z((128, 1)))
        p.add("kn%d" % l, z((128, 1)))
        p.add("hnorm%d" % l, z((128, 4)))
        p.add("rnorm%d" % l, z((128, 4)))
        p.add("convw%d" % l, z((128, 6)))
        p.add("rb%d" % l, z((128, 20)))
        for d in range(2):
            p.add("hlb%d_%d" % (l, d), z((128, 4)))
            p.add("rdl%d_%d" % (l, d), z((128, 4)))
    return p


def pack_pv(inp, nseq):
    p = Pack(128)
    for l in range(DEPTH):
        p.add("nmix%d" % l, fm(inp["norm_mix"][l]))
        p.add("nffn%d" % l, fm(inp["norm_ffn"][l]))
        p.add("adab%d" % l, fm(inp["ada_b"][l]))
        p.add("qn%d" % l, np.tile(inp["q_norm"][l], 2).reshape(128, 1))
        p.add("kn%d" % l, np.tile(inp["k_norm"][l], 2).reshape(128, 1))
        p.add("hnorm%d" % l, hd(inp["hgrn_norm"][l]))
        p.add("rnorm%d" % l, hd(inp["ret_norm"][l]))
        p.add("convw%d" % l, np.stack([fm(inp["conv_w"][l, j]) for j in range(3)], axis=1))
        rb = np.concatenate([inp["router_group_b"][l], inp["router_expert_b"][l]])
        p.add("rb%d" % l, np.broadcast_to(rb[None, :], (128, 20)))
        for d in range(2):
            p.add("hlb%d_%d" % (l, d), hd(inp["hgrn_lb_logits"][l, d]))
            p.add("rdl%d_%d" % (l, d), np.broadcast_to(inp["ret_decay_logit"][l, d][None, :], (128, 4)))
    return p.array()


def rope_tables():
    t = np.arange(T)
    row = (t // 64).astype(np.float32)
    col = (t % 64).astype(np.float32)
    nf = 16
    inv = (np.float32(10000.0) ** (-np.arange(nf, dtype=np.float32) / nf)).astype(np.float32)
    ang = np.concatenate([row[:, None] * inv, col[:, None] * inv], axis=-1)
    cos = np.repeat(np.cos(ang), 2, axis=1).T
    sin = np.repeat(np.sin(ang), 2, axis=1).T
    return np.ascontiguousarray(np.stack([np.tile(cos, (2, 1)), np.tile(sin, (2, 1))]), dtype=np.float32)


def make_consts():
    c = Pack(128)
    c.add("ident", np.eye(128))
    p = np.arange(128)
    c.add("bo64", (p[:, None] // 64 == p[None, :] // 64).astype(np.float32))
    rT = np.zeros((128, 128), np.float32)
    for i in range(64):
        rT[2 * i + 1, 2 * i] = -1.0
        rT[2 * i, 2 * i + 1] = 1.0
    c.add("rrotT", rT)
    sp = p[:, None]
    sc = p[None, :]
    same = (sp // 64 == sc // 64)
    mid = (sc // 64) * 64 + 31
    c.add("M0", same * ((sp <= sc).astype(np.float32) - (sp <= mid).astype(np.float32)))
    c.add("M1", same * ((sp >= sc).astype(np.float32) - (sp >= mid).astype(np.float32)))
    c.add("A0", (same & (sp <= sc)).astype(np.float32))
    c.add("A1", (same & (sp >= sc)).astype(np.float32))
    c.add("D0", (same & (sp > sc)).astype(np.float32))
    c.add("D1", (same & (sp < sc)).astype(np.float32))
    c.add("K0", (same & (sp <= sc)).astype(np.float32))
    c.add("K1", (same & (sp >= sc)).astype(np.float32))
    c.add("Sel", (p[:, None] // 64 == np.arange(2)[None, :]).astype(np.float32))
    return c


SB0 = 16512
OFF_XT = SB0
OFF_HT = OFF_XT + 8 * NT * 4
OFF_CONST = OFF_HT + 8 * NT * 2
CONST_BYTES = 14336
OFF_SCR = OFF_CONST + CONST_BYTES
SB_END = 229344 - 3 * 2048


class Rot:
    def __init__(self, bufs):
        self.bufs, self.i = bufs, 0

    def next(self):
        b = self.bufs[self.i % len(self.bufs)]
        self.i += 1
        return b


class MK:
    def __init__(self, nseq=2, nlayers=2, debug=None, stages=("mix", "moe")):
        self.nseq, self.nlayers, self.debug, self.stages = nseq, nlayers, debug, stages

    def build(self):
        nc = bass.Bass("TRN2", target_bir_lowering=False)
        self.nc = nc
        P = Prog(nc)
        self.P = P
        k = K(P)
        self.k = k
        nseq = self.nseq
        self.pvl = pv_layout()
        self.cstl = make_consts()
        dt = nc.dram_tensor
        self.x_d = dt("x", [nseq, T, D], F32, kind="ExternalInput").ap()
        self.ctx_d = dt("ctx", [nseq, LC, D], F32, kind="ExternalInput").ap()
        self.cc_d = dt("cc", [128, 24], F32, kind="ExternalInput").ap()
        self.pv_d = dt("pv", [128, self.pvl.cols], F32, kind="ExternalInput").ap()
        self.cst_d = dt("cst", [128, self.cstl.cols], F32, kind="ExternalInput").ap()
        self.adaw_d = dt("ada_w", [DEPTH, D, 6 * D], F32, kind="ExternalInput").ap()
        self.win_d = dt("w_in", [DEPTH, D, INW], F32, kind="ExternalInput").ap()
        self.wout_d = dt("w_out", [DEPTH, D, D], F32, kind="ExternalInput").ap()
        self.rope_d = dt("rope", [2, 128, T], F32, kind="ExternalInput").ap()
        self.hl_d = dt("hl", [DEPTH, 2, 256], F32, kind="ExternalInput").ap()
        self.rl_d = dt("rl", [DEPTH, 2, 256], F32, kind="ExternalInput").ap()
        self.rw_d = dt("rw", [DEPTH, 128, 8, 20], F32, kind="ExternalInput").ap()
        self.wg_d = dt("wg", [DEPTH, 16, D, 512], F32, kind="ExternalInput").ap()
        self.wu_d = dt("wu", [DEPTH, 16, D, 512], F32, kind="ExternalInput").ap()
        self.wd_d = dt("wd", [DEPTH, 16, 512, D], F32, kind="ExternalInput").ap()
        self.out_d = dt("out", [nseq, T, D], F32, kind="ExternalOutput").ap()
        if self.debug:
            name, shape, dtype = self.debug
            self.dbg_d = dt("dbg", list(shape), dtype, kind="ExternalOutput").ap()

        self.xT = P.sb("xT", [128, 8, NT], F32, OFF_XT, group="xT")
        self.hT = P.sb("hT", [128, 8, NT], BF16, OFF_HT, group="hT")
        o = OFF_CONST
        self.pv = P.sb("pv", [128, self.pvl.cols], F32, o, group="pv"); o = self.pv.end
        self.cst = P.sb("cst", [128, self.cstl.cols], F32, o, group="cst"); o = self.cst.end
        self.onesb = P.sb("onesb", [128, 128], BF16, o, group="onesb"); o = self.onesb.end
        self.scc = P.sb("scc", [128, 8, 3], F32, o, group="scc"); o = self.scc.end
        self.mod = [P.sb("mod%d" % l, [128, 48, 3], F32, o + l * 576, group="mod%d" % l) for l in range(DEPTH)]
        o += 2 * 576
        self.gpm = [P.sb("gpm%d" % l, [128, 8, 3], F32, o + l * 96, group="gpm%d" % l) for l in range(DEPTH)]
        o += 2 * 96
        self.gpf = [P.sb("gpf%d" % l, [128, 8, 3], F32, o + l * 96, group="gpf%d" % l) for l in range(DEPTH)]
        o += 2 * 96
        assert o <= OFF_CONST + CONST_BYTES, o - OFF_CONST
        self.const_end = o
        self.psb = [P.ps("psb%d" % i) for i in range(8)]
        self.psrot = Rot(self.psb)
        self.toks_out = []

        self.setup()
        for s in range(nseq):
            self.load_x(s)
            for l in range(self.nlayers):
                self.layer(s, l)
            self.store_x(s)
        P.wait_all("sp", self.toks_out)
        P.emit()
        return nc

    def pvv(self, name):
        o, n = self.pvl.ent[name]
        return self.pv[:, o:o + n]

    def pvr(self, name, rows):
        o, n = self.pvl.ent[name]
        return self.pv[0:rows, o:o + n]

    def cv(self, name):
        o, n = self.cstl.ent[name]
        return self.cst[:, o:o + n]

    def setup(self):
        P, k = self.P, self.k
        k.load(self.pv[:], self.pv_d)
        k.load(self.cst[:], self.cst_d)
        k.memset(self.onesb[:], 1.0)
        cc = P.sb("cc_t", [128, 8, 3], F32, OFF_SCR, group="scr")
        k.load(cc[:], self.cc_d.rearrange("p (k c) -> p k c", c=3))
        k.act(self.scc[:], cc[:], AF.Silu)
        NB = 768
        blks = [P.sb("adaw%d" % i, [128, 8, NB], F32, OFF_SCR + 1024 + i * 8 * NB * 4, group="scr") for i in range(2)]
        rot = Rot(blks)
        for l in range(self.nlayers):
            ps = self.psb[l]
            for b in range(6 * D // NB):
                blk = rot.next()
                src = self.adaw_d[l].rearrange("(k p) n -> p k n", p=128)[:, :, b * NB:(b + 1) * NB]
                k.load(blk[:], src)
                for jj in range(NB // 128):
                    j = b * (NB // 128) + jj
                    for kk in range(8):
                        k.mm(ps[:, j * 3:j * 3 + 3], blk[:, kk, jj * 128:(jj + 1) * 128], self.scc[:, kk, :],
                             start=(kk == 0), stop=(kk == 7))
            pv3 = ps[:, 0:144]
            ab = self.pvv("adab%d" % l)
            k.tt(self.mod[l][:], pv3.w(pv3.ap.rearrange("p (j c) -> p j c", c=3)),
                 ab.w(ab.ap.unsqueeze(2).broadcast_to([128, 48, 3])), ALU.add)
            for (gp, nm, m0) in ((self.gpm[l], "nmix%d" % l, 8), (self.gpf[l], "nffn%d" % l, 32)):
                g = self.pvv(nm)
                k.stt(gp[:], self.mod[l][:, m0:m0 + 8, :], 1.0, g.w(g.ap.unsqueeze(2).broadcast_to([128, 8, 3])),
                      ALU.add, ALU.mult)

    def load_x(self, s):
        P, k = self.P, self.k
        bufs = [P.sb("xtm%d" % i, [128, D], F32, OFF_SCR + i * 4096, group="scr") for i in range(3)]
        rot = Rot(bufs)
        ident = self.cv("ident")
        for j in range(NTILE):
            b = rot.next()
            if j < 2:
                src = self.ctx_d[s, j * 128:(j + 1) * 128, :]
            else:
                src = self.x_d[s, (j - 2) * 128:(j - 1) * 128, :]
            k.load(b[:], src)
            for half in range(2):
                ps = self.psrot.next()
                for c in range(4):
                    kk = half * 4 + c
                    k.transpose(ps[:, c * 128:(c + 1) * 128], b[:, kk * 128:(kk + 1) * 128], ident)
                dst = self.xT[:, half * 4:half * 4 + 4, j * 128:(j + 1) * 128]
                srcv = ps[:]
                k.copy(dst, srcv.w(srcv.ap.rearrange("p (c t) -> p c t", c=4)), eng=("act" if half else "dve"))

    def store_x(self, s):
        P, k = self.P, self.k
        bufs = [P.sb("xo%d" % i, [128, D], F32, OFF_SCR + i * 4096, group="scr") for i in range(3)]
        rot = Rot(bufs)
        ident = self.cv("ident")
        for j in range(2, NTILE):
            b = rot.next()
            for half in range(2):
                ps = self.psrot.next()
                for c in range(4):
                    kk = half * 4 + c
                    k.transpose(ps[:, c * 128:(c + 1) * 128], self.xT[:, kk, j * 128:(j + 1) * 128], ident)
                k.copy(b[:, half * 512:(half + 1) * 512], ps[:], eng=("act" if half else "dve"))
            tok = k.store(self.out_d[s, (j - 2) * 128:(j - 1) * 128, :], b[:])
            self.toks_out.append(tok)

    def tblocks(self, with_ctx=True):
        bl = []
        if with_ctx:
            bl.append((0, LC, True))
        for i in range(T // 512):
            bl.append((LC + i * 512, 512, False))
        return bl

    def norm_mod(self, s, l, which, scr_off):
        P, k = self.P, self.k
        gp = (self.gpm if which == 0 else self.gpf)[l]
        sh0 = 0 if which == 0 else 24
        o = scr_off
        sq = Rot([P.sb("nsq%d" % i, [128, 512], BF16, o + i * 1024, group="scr") for i in range(3)])
        o += 3 * 1024
        lnv = P.sb("nln", [128, 512], F32, o, group="scr"); o += 2048
        rstd = Rot([P.sb("nrstd%d" % i, [128, 512], F32, o + i * 2048, group="scr") for i in range(2)])
        o += 2 * 2048
        tmp = Rot([P.sb("ntmp%d" % i, [128, 512], F32, o + i * 2048, group="scr") for i in range(3)])
        o += 3 * 2048
        with_ctx = True
        for (t0, n, isctx) in self.tblocks(with_ctx):
            col = 2 if isctx else s
            ps = self.psrot.next()
            for kk in range(8):
                q = sq.next()
                k.act(q[:, 0:n], self.xT[:, kk, t0:t0 + n], AF.Square)
                k.mm(ps[:, 0:n], self.onesb[:], q[:, 0:n], start=(kk == 0), stop=(kk == 7))
            k.act(lnv[:, 0:n], ps[:, 0:n], AF.Ln, bias=EPS, scale=1.0 / D)
            r = rstd.next()
            k.act(r[:, 0:n], lnv[:, 0:n], AF.Exp, scale=-0.5)
            for kk in range(8):
                t = tmp.next()
                k.stt(t[:, 0:n], self.xT[:, kk, t0:t0 + n], gp[:, kk, col:col + 1], r[:, 0:n], ALU.mult, ALU.mult)
                k.act(self.hT[:, kk, t0:t0 + n], t[:, 0:n], AF.Identity, bias=self.mod[l][:, sh0 + kk, col:col + 1])
        return o

    def mark(self, label):
        if not hasattr(self, "marks"):
            self.marks = []
        self.marks.append((label, dict(self.P.cnt)))

    def layer(self, s, l):
        with_ctx_out = l < DEPTH - 1
        self.mark("s%d l%d norm1" % (s, l))
        o = self.norm_mod(s, l, 0, OFF_SCR)
        if self.debug and self.debug[0] == "hT" and s == 0 and l == 0:
            tok = self.k.store(self.dbg_d, self.hT[:])
            self.toks_out.append(tok)
        if "conv" in self.stages:
            self.mark("s%d l%d conv" % (s, l))
            self.conv_group(s, l, with_ctx_out)
        if "hgrn" in self.stages:
            self.mark("s%d l%d hgrn" % (s, l))
            self.scan_group(s, l, with_ctx_out, "hgrn")
        if "ret" in self.stages:
            self.mark("s%d l%d ret" % (s, l))
            self.scan_group(s, l, with_ctx_out, "ret")
        if "attn" in self.stages:
            self.mark("s%d l%d attn" % (s, l))
            self.attn_group(s, l, with_ctx_out)
        if "moe" in self.stages:
            self.mark("s%d l%d norm2" % (s, l))
            self.norm_mod(s, l, 1, OFF_SCR)
            self.mark("s%d l%d moe" % (s, l))
            self.moe(s, l, with_ctx_out)
        self.mark("s%d l%d end" % (s, l))

    def load_w(self, dst, src_ap):
        self.P.dma("pool", lambda e: e.dma_start(out=dst.ap, in_=src_ap), writes=[dst])

    def proj_fm(self, out, wg, col0, m, t0, n):
        k = self.k
        for kk in range(8):
            k.mm(out, wg[:, kk, col0:col0 + m], self.hT[:, kk, t0:t0 + n], start=(kk == 0), stop=(kk == 7))

    def wout_apply(self, s, l, wo, nk, kp, mix, t0, n, isctx):
        k = self.k
        col = 2 if isctx else s
        for oc in range(8):
            ps = self.psrot.next()
            for c in range(nk):
                k.mm(ps[:, 0:n], wo[0:kp, c, oc * 128:(oc + 1) * 128], mix[0:kp, c, 0:n], start=(c == 0), stop=(c == nk - 1))
            if getattr(self, "conv_upto", 9) < 7:
                continue
            self.resid_add(oc, t0, n, ps, self.mod[l][:, 16 + oc, col:col + 1])

    def resid_add(self, oc, t0, n, ps, gate):
        k = self.k
        if not hasattr(self, "ra_rot"):
            self.ra_rot = Rot([self.P.sb("ra%d" % i, [128, 512], F32, 229344 - (i + 1) * 2048, group="scr") for i in range(3)])
        t = self.ra_rot.next()
        k.act(t[:, 0:n], ps[:, 0:n], AF.Copy, scale=gate)
        k.tt(self.xT[:, oc, t0:t0 + n], self.xT[:, oc, t0:t0 + n], t[:, 0:n], ALU.add)

    def conv_group(self, s, l, with_ctx_out):
        P, k = self.P, self.k
        o = OFF_SCR
        wg = P.sb("cv_wg", [128, 8, 768], BF16, o, group="scr"); o = wg.end
        wo = P.sb("cv_wo", [128, 2, 1024], BF16, o, group="scr"); o = wo.end
        uT = P.sb("cv_u", [128, 2, NT], F32, o, group="scr"); o = uT.end
        tmpc = Rot([P.sb("cv_tc%d" % i, [128, 512], F32, o + i * 2048, group="scr") for i in range(2)]); o += 4096
        acc = Rot([P.sb("cv_acc%d" % i, [128, 512], F32, o + i * 2048, group="scr") for i in range(2)]); o += 4096
        mixb = Rot([P.sb("cv_mix%d" % i, [128, 2, 512], BF16, o + i * 2048, group="scr") for i in range(2)]); o += 4096
        assert o <= SB_END
        self.load_w(wg[:], self.win_d[l].rearrange("(k p) n -> p k n", p=128)[:, :, 1280:2048])
        self.load_w(wo[:], self.wout_d[l][256:512, :].rearrange("(c p) n -> p c n", p=128))
        blocks = self.tblocks(with_ctx_out)
        upto = getattr(self, "conv_upto", 9)
        if upto < 2:
            return
        for (t0, n, isctx) in blocks:
            for c in range(2):
                psC = self.psrot.next()
                self.proj_fm(psC[:, 0:n], wg, 256 + c * 128, 128, t0, n)
                psH = self.psrot.next()
                self.proj_fm(psH[:, 0:n], wg, 512 + c * 128, 128, t0, n)
                tc_ = tmpc.next()
                k.copy(tc_[:, 0:n], psC[:, 0:n], eng="act")
                k.tt(uT[:, c, t0:t0 + n], tc_[:, 0:n], psH[:, 0:n], ALU.mult)
        if upto < 3:
            return
        cw = self.pvv("convw%d" % l)
        co = self.pvl.ent["convw%d" % l][0]
        for (t0, n, isctx) in blocks:
            r0, r1 = (0, LC) if isctx else (LC, NT)
            mb = mixb.next()
            for c in range(2):
                psB = self.psrot.next()
                self.proj_fm(psB[:, 0:n], wg, c * 128, 128, t0, n)
                a = acc.next()
                w0 = self.pv[:, co + 0 + c:co + 1 + c]
                w1 = self.pv[:, co + 2 + c:co + 3 + c]
                w2 = self.pv[:, co + 4 + c:co + 5 + c]
                k.ts(a[:, 0:n], uT[:, c, t0:t0 + n], w1, ALU.mult)
                if upto < 4:
                    continue
                lo = max(t0, r0 + 1)
                k.stt(a[:, lo - t0:n], uT[:, c, lo - 1:t0 + n - 1], w0, a[:, lo - t0:n], ALU.mult, ALU.add)
                hi = min(t0 + n, r1 - 1)
                k.stt(a[:, 0:hi - t0], uT[:, c, t0 + 1:hi + 1], w2, a[:, 0:hi - t0], ALU.mult, ALU.add)
                if upto < 5:
                    continue
                k.tt(mb[:, c, 0:n], a[:, 0:n], psB[:, 0:n], ALU.mult)
            if upto < 6:
                continue
            self.wout_apply(s, l, wo, 2, 128, mb, t0, n, isctx)

    def scan_group(self, s, l, with_ctx_out, kind):
        P, k = self.P, self.k
        hg = kind == "hgrn"
        ncol = 1280 if hg else 1024
        c0 = 0 if hg else 2048
        row0 = 0 if hg else 512
        QC, GC = 0, (1024 if hg else 768)
        VC = 256 if hg else 512
        o = OFF_SCR
        wg = P.sb("sc_wg", [128, 8, ncol], BF16, o, group="scr"); o = wg.end
        wo = P.sb("sc_wo", [64, 4, 1024], BF16, o, group="scr"); o = wo.end
        oacc = P.sb("sc_oacc", [64, 4, NT], BF16, o, group="scr"); o = oacc.end
        def T_(nm, shape, dt_):
            nonlocal o
            t = P.sb("sc_" + nm, list(shape), dt_, o, group="scr"); o = t.end
            return t
        lbt = [T_("lbt%d" % d, (128, 256), F32) for d in range(2)]
        omt = [T_("omt%d" % d, (128, 256), F32) for d in range(2)] if hg else None
        lbf = T_("lbf", (64, 2, 4), F32)
        omf = T_("omf", (64, 2, 4), F32)
        zt = T_("zt", (128, 256), F32); lf = T_("lf", (128, 256), F32)
        kdec = T_("kdec", (128, 256), BF16); vt = T_("vt", (128, 256), BF16)
        eq = T_("eq", (64, 512), F32); ek = T_("ek", (64, 512), F32); ea = T_("ea", (64, 512), F32)
        ed = T_("ed", (128, 256), F32); abv = T_("abv", (64, 4, 2), F32)
        kTf = T_("kTf", (64, 512), F32)
        qTf = None if hg else T_("qTf", (64, 512), F32)
        qtil = T_("qtil", (64, 4, 128), BF16); ktil = T_("ktil", (64, 4, 128), BF16); qabs = T_("qabs", (64, 4, 128), BF16)
        scm = T_("scm", (128, 4, 128), BF16)
        sct = T_("sct", (128, 4, 64), F32)
        kt = P.sb("sc_kt", [128, 256], F32, sct.off, group="scr")
        S = T_("S", (64, 4, 64), F32); Sb = T_("Sb", (64, 4, 64), BF16)
        osum = P.sb("sc_osum", [64, 512], F32, (ea.off if hg else qTf.off), group="scr")
        sg = P.sb("sc_sg", [64, 512], F32, kTf.off, group="scr")
        if hg:
            rsd = P.sb("sc_rsd", [64, 512], F32, eq.off, group="scr")
            sqb = P.sb("sc_sqb", [64, 512], BF16, ek.off, group="scr")
        else:
            sqb = T_("sqb", (64, 512), BF16); rsd = T_("rsd", (64, 512), F32)
        tbuf = T_("tbuf", (64, 4, 512), BF16)
        Sab = T_("Sab", (64, 4, 64), F32)
        ones64 = T_("ones64", (64, 64), BF16)
        assert o <= SB_END, o
        win = self.win_d[l].rearrange("(k p) n -> p k n", p=128)
        self.load_w(wg[:], win[:, :, c0:c0 + ncol])
        self.load_w(wo[:], self.wout_d[l][row0:row0 + 256, :].rearrange("(h d) n -> d h n", d=64))
        k.memset(ones64[:], 1.0)
        k.memset(scm[:], 0.0)
        b = self.psb
        gain = self.pvv(("hnorm%d" if hg else "rnorm%d") % l)
        for d in range(2):
            if hg:
                if l == 0:
                    k.memset(lbt[d][:], 0.0)
                    k.memset(lbf[:, d, :], 0.0)
                else:
                    k.load(lbt[d][:], self.hl_d[1, d:d + 1, :].partition_broadcast(128).rearrange("p a n -> p (a n)"))
                    k.load(zt[:], self.hl_d[0, d:d + 1, :].partition_broadcast(128).rearrange("p a n -> p (a n)"))
                    k.tt(lbt[d][:], lbt[d][:], zt[:], ALU.subtract)
                    k.act(lbt[d][:], lbt[d][:], AF.Sigmoid)
                    k.tt(lbf[:, d, :], self.pvr("hlb1_%d" % d, 64), self.pvr("hlb0_%d" % d, 64), ALU.subtract)
                    k.act(lbf[:, d, :], lbf[:, d, :], AF.Sigmoid)
                k.ts(omt[d][:], lbt[d][:], -1.0, ALU.mult, 1.0, ALU.add)
                k.ts(omf[:, d, :], lbf[:, d, :], -1.0, ALU.mult, 1.0, ALU.add)
            else:
                k.load(lbt[d][:], self.rl_d[l, d:d + 1, :].partition_broadcast(128).rearrange("p a n -> p (a n)"))
                k.act(lbt[d][:], lbt[d][:], AF.Exp, scale=-1.0)
                k.act(lbt[d][:], lbt[d][:], AF.Ln, bias=1.0)
                k.ts(lbt[d][:], lbt[d][:], -1.0, ALU.mult)

        import os
        hup = int(os.environ.get("HG_UPTO", "9"))
        if hup < 2:
            return

        cb = {}
        for nm in ("M0", "M1", "A0", "A1", "D0", "D1"):
            cb[nm] = T_("cb_" + nm, (128, 128), BF16)
            k.copy(cb[nm][:], self.cv(nm))
        cb["Sel"] = T_("cb_Sel", (128, 2), BF16)
        k.copy(cb["Sel"][:], self.cv("Sel"))
        lfh = T_("lfh", (128, 256), BF16)
        lfl = T_("lfl", (128, 256), BF16)
        lfr = zt
        kdec2 = T_("kdec2", (128, 256), BF16); vt2 = T_("vt2", (128, 256), BF16)
        qabs2 = T_("qabs2", (64, 4, 128), BF16); scm2 = T_("scm2", (128, 4, 128), BF16)
        abv2 = T_("abv2", (64, 4, 2), F32) if hg else abv
        sets = [(kdec, vt, qabs, scm, abv), (kdec2, vt2, qabs2, scm2, abv2)]
        k.memset(scm2[:], 0.0)
        assert o <= SB_END, o

        def decay_prep(d, lfv, abv, have_hi=False):
            M, A, Dm, Sel = cb["M%d" % d], cb["A%d" % d], cb["D%d" % d], cb["Sel"]
            if not have_hi:
                k.copy(lfh[:], lfv, eng="act")
            k.tt(lfl[:], lfv, lfh[:], ALU.subtract)
            for i, part in enumerate((lfh, lfl)):
                k.mm(b[3][:, 0:256], Dm[:], part[:], start=(i == 0), stop=(i == 1))
            for h in range(4):
                hc = slice(h * 64, (h + 1) * 64)
                for i, part in enumerate((lfh, lfl)):
                    k.mm(b[4][0:64, h * 128:(h + 1) * 128], part[:, hc], M[:], start=(i == 0), stop=(i == 1))
                for i, part in enumerate((lfh, lfl)):
                    k.mm(b[5][0:64, h * 128:(h + 1) * 128], part[:, hc], A[:], start=(i == 0), stop=(i == 1))
            for h in range(4):
                hc = slice(h * 64, (h + 1) * 64)
                for i, part in enumerate((lfh, lfl)):
                    k.mm(b[3][0:64, 256 + h * 2:258 + h * 2], part[:, hc], Sel[:], start=(i == 0), stop=(i == 1))
            k.act(ed[:], b[3][:, 0:256], AF.Exp)
            k.act(eq[:], b[4][0:64, :], AF.Exp)
            k.act(ek[:], b[4][0:64, :], AF.Exp, scale=-1.0)
            k.act(ea[:], b[5][0:64, :], AF.Exp)
            av = b[3][0:64, 256:264]
            k.act(abv[:], av.w(av.ap.rearrange("p (h u) -> p h u", u=2)), AF.Exp)

        def v3(t):
            return t.w(t.ap.rearrange("p (h t) -> p h t", h=4))

        for d in range(2):
            order = list(range(NTILE)) if d == 0 else [1, 0] + list(range(NTILE - 1, 1, -1))
            uo = (0, 1) if d == 0 else (1, 0)
            k.memset(S[:], 0.0)
            k.memset(Sb[:], 0.0)
            mask = self.cv("K%d" % d)
            if not hg:
                decay_prep(d, lbt[d][:], abv)
                k.ts(eq[:], eq[:], 0.125, ALU.mult)
                k.ts(ea[:], ea[:], 0.125, ALU.mult)
            def front_a(j, p):
                kdec, vt, qabs, scm, abv_p = sets[p]
                t0 = j * 128
                kcol = (512 + d * 256) if hg else 256
                for kk in range(8):
                    k.mm(b[2][:, 0:256], self.hT[:, kk, t0:t0 + 128], wg[:, kk, kcol:kcol + 256], start=(kk == 0), stop=(kk == 7))
                for kk in range(8):
                    k.mm(b[2][:, 256:512], self.hT[:, kk, t0:t0 + 128], wg[:, kk, VC:VC + 256], start=(kk == 0), stop=(kk == 7))
                if hg:
                    k.act(zt[:], b[2][:, 0:256], AF.Sigmoid)
                    k.act(kt[:], b[2][:, 0:256], AF.Sigmoid, scale=-1.0)
                    k.copy(vt[:], b[2][:, 256:512], eng="act")
                    if l > 0:
                        k.tt(zt[:], zt[:], omt[d][:], ALU.mult)
                        k.tt(zt[:], zt[:], lbt[d][:], ALU.add)
                        k.tt(kt[:], kt[:], omt[d][:], ALU.mult)
                    k.act(lf[:], zt[:], AF.Ln)
                    k.act(lfh[:], zt[:], AF.Ln)
                else:
                    k.copy(vt[:], b[2][:, 256:512], eng="act")
                    k.tt(kdec[:], b[2][:, 0:256], ed[:], ALU.mult)

            def front_b(j, p):
                kdec, vt, qabs, scm, abv_p = sets[p]
                isctx = j < 2
                need_out = (not isctx) or with_ctx_out
                t0 = j * 128
                kcol = (512 + d * 256) if hg else 256
                if hg:
                    decay_prep(d, lf[:], abv_p, have_hi=True)
                    k.tt(kdec[:], kt[:], ed[:], ALU.mult)
                if not need_out:
                    return
                for h in range(4):
                    for kk in range(8):
                        k.mm(b[0][0:64, h * 128:(h + 1) * 128], wg[:, kk, QC + h * 64:QC + (h + 1) * 64],
                             self.hT[:, kk, t0:t0 + 128], start=(kk == 0), stop=(kk == 7))
                for h in range(4):
                    for kk in range(8):
                        k.mm(b[1][0:64, h * 128:(h + 1) * 128], wg[:, kk, kcol + h * 64:kcol + (h + 1) * 64],
                             self.hT[:, kk, t0:t0 + 128], start=(kk == 0), stop=(kk == 7))
                if hg:
                    k.act(kTf[:], b[1][0:64, :], AF.Sigmoid, scale=-1.0)
                    if l > 0:
                        omb = omf[:, d, :]
                        k.tt(v3(kTf[:]), v3(kTf[:]), omb.w(omb.ap.unsqueeze(2).broadcast_to([64, 4, 128])), ALU.mult)
                    ksrc = kTf[:]
                else:
                    ksrc = b[1][0:64, :]
                qsrc = b[0][0:64, :]
                k.tt(qtil[:].w(qtil[:].ap.rearrange("p h t -> p (h t)")), qsrc, eq[:], ALU.mult)
                k.tt(qabs[:].w(qabs[:].ap.rearrange("p h t -> p (h t)")), qsrc, ea[:], ALU.mult)
                k.tt(ktil[:].w(ktil[:].ap.rearrange("p h t -> p (h t)")), ksrc, ek[:], ALU.mult)
                mo = self.cstl.ent["K%d" % d][0]
                for u in range(2):
                    us = slice(u * 64, (u + 1) * 64)
                    for h in range(4):
                        k.mm(b[6][us, h * 128 + u * 64:h * 128 + (u + 1) * 64], ktil[:, h, us], qtil[:, h, us])
                for u in range(2):
                    us = slice(u * 64, (u + 1) * 64)
                    sv = b[6][us, :]
                    sv = sv.w(sv.ap.rearrange("p (h t) -> p h t", h=4)[:, :, u * 64:(u + 1) * 64])
                    mk_ = self.cst[us, mo + u * 64:mo + (u + 1) * 64]
                    mkb = mk_.w(mk_.ap.unsqueeze(1).broadcast_to([64, 4, 64]))
                    if hg:
                        k.ts(sct[us, :, :], sv, 1e30, ALU.min, -1e30, ALU.max)
                        k.tt(scm[us, :, us], sct[us, :, :], mkb, ALU.mult)
                    else:
                        k.tt(scm[us, :, us], sv, mkb, ALU.mult)

            def back(j, p):
                kdec, vt, qabs, scm, abv_p = sets[p]
                isctx = j < 2
                need_out = (not isctx) or with_ctx_out
                t0 = j * 128
                for ui, u in enumerate(uo):
                    us = slice(u * 64, (u + 1) * 64)
                    for h in range(4):
                        k.mm(b[5 - ui][0:64, h * 64:(h + 1) * 64], kdec[us, h * 64:(h + 1) * 64],
                             vt[us, h * 64:(h + 1) * 64])
                for ui, u in enumerate(uo):
                    us = slice(u * 64, (u + 1) * 64)
                    if need_out:
                        for h in range(4):
                            oreg = b[7][0:64, h * 128 + u * 64:h * 128 + (u + 1) * 64]
                            k.mm(oreg, vt[:, h * 64:(h + 1) * 64], scm[:, h, us], start=True, stop=False)
                            k.mm(oreg, Sb[:, h, :], qabs[:, h, us], start=False, stop=True)
                    ab = abv_p[:, :, u:u + 1]
                    kvv = b[5 - ui][0:64, 0:256]
                    kvv = kvv.w(kvv.ap.rearrange("p (h e) -> p h e", h=4))
                    k.tt(Sab[:], S[:], ab.w(ab.ap.broadcast_to([64, 4, 64])), ALU.mult)
                    k.tt(Sb[:], Sab[:], kvv, ALU.add)
                    k.tt(S[:], Sab[:], kvv, ALU.add)
                if not need_out:
                    return
                o7 = b[7][0:64, :]
                if d == 0:
                    k.copy(oacc[:, :, t0:t0 + 128], o7.w(o7.ap.rearrange("p (h t) -> p h t", h=4)), eng="act")
                    return
                if isctx:
                    blk0, nblk = 0, LC
                else:
                    blk0 = LC + ((t0 - LC) // 512) * 512
                    nblk = 512
                off = t0 - blk0
                k.tt(v3(osum[:]), o7.w(o7.ap.rearrange("p (h t) -> p h t", h=4)), oacc[:, :, t0:t0 + 128], ALU.add)
                k.act(sqb[:], osum[:], AF.Square)
                k.mm(b[0][0:64, :], ones64[:], sqb[:])
                k.act(rsd[:], b[0][0:64, :], AF.Ln, bias=EPS, scale=1.0 / 64)
                k.act(rsd[:], rsd[:], AF.Exp, scale=-0.5)
                k.tt(osum[:], osum[:], rsd[:], ALU.mult)
                g64 = self.pvr(("hnorm%d" if hg else "rnorm%d") % l, 64)
                k.tt(tbuf[:, :, off:off + 128], v3(osum[:]), g64.w(g64.ap.unsqueeze(2).broadcast_to([64, 4, 128])), ALU.mult)
                if t0 != blk0:
                    return
                for h in range(4):
                    ps = b[h % 2]
                    for kk in range(8):
                        k.mm(ps[0:64, 0:nblk], wg[:, kk, GC + h * 64:GC + (h + 1) * 64],
                             self.hT[:, kk, blk0:blk0 + nblk], start=(kk == 0), stop=(kk == 7))
                    k.act(sg[:, 0:nblk], ps[0:64, 0:nblk], AF.Silu)
                    k.tt(tbuf[:, h, 0:nblk], tbuf[:, h, 0:nblk], sg[:, 0:nblk], ALU.mult)
                col = 2 if isctx else s
                for oc in range(8):
                    ps = b[2 + oc % 2]
                    for h in range(4):
                        k.mm(ps[:, 0:nblk], wo[:, h, oc * 128:(oc + 1) * 128], tbuf[:, h, 0:nblk], start=(h == 0), stop=(h == 3))
                    self.resid_add(oc, blk0, nblk, ps, self.mod[l][:, 16 + oc, col:col + 1])

            front_a(order[0], 0)
            front_b(order[0], 0)
            for i, j in enumerate(order):
                if i + 1 < len(order):
                    front_a(order[i + 1], (i + 1) % 2)
                back(j, i % 2)
                if i + 1 < len(order):
                    front_b(order[i + 1], (i + 1) % 2)

    def _kv_view(self, b, need_out):
        v = b[5][0:64, 0:256] if not need_out else b[3][0:64, 272:528]
        return v.w(v.ap.rearrange("p (h e) -> p h e", h=4))

    def attn_group(self, s, l, with_ctx_out):
        P, k = self.P, self.k
        o = OFF_SCR
        wg = P.sb("at_wg", [128, 8, 512], BF16, o, group="scr"); o = wg.end
        wo = P.sb("at_wo", [128, 2, 1024], BF16, o, group="scr"); o = wo.end
        QT = P.sb("at_QT", [128, 2, NT], BF16, o, group="scr"); o = QT.end
        KT = P.sb("at_KT", [128, NT], BF16, o, group="scr"); o = KT.end
        Vd = P.sb("at_V", [128, NTILE, 2, 128], BF16, o, group="scr"); o = Vd.end
        cs = P.sb("at_cs", [128, 2, T], F32, o, group="scr"); o = cs.end
        bo = P.sb("at_bo", [128, 128], BF16, o, group="scr"); o = bo.end
        def rot(nm, dt_, nb, shape=(128, 512)):
            nonlocal o
            sz = int(np.prod(shape[1:])) * ESZ[dt_]
            r = Rot([P.sb("at_%s%d" % (nm, i), list(shape), dt_, o + i * sz, group="scr") for i in range(nb)])
            o += nb * sz
            return r
        sqr = rot("sq", BF16, 2); rsr = rot("rs", F32, 2); kgr = rot("kg", F32, 2)
        t1r = rot("t1", F32, 2); t2r = rot("t2", F32, 2); ptr = rot("pt", BF16, 3)
        rdr = rot("rd", F32, 2); mxr = rot("mx", BF16, 2, (128, 2, 512))
        assert o <= SB_END, o
        win = self.win_d[l].rearrange("(k p) n -> p k n", p=128)
        for i, h in enumerate((0, 2, 1, 3)):
            self.load_w(wg[:, :, i * 64:(i + 1) * 64], win[:, :, 3072 + h * 64:3072 + (h + 1) * 64])
            c, hp = i // 2, i % 2
            self.load_w(wo[hp * 64:(hp + 1) * 64, c, :], self.wout_d[l][768 + h * 64:768 + (h + 1) * 64, :])
        self.load_w(wg[:, :, 256:512], win[:, :, 3328:3584])
        k.load(cs[:], self.rope_d.rearrange("c p t -> p c t"))
        k.copy(bo[:], self.cv("bo64"))
        rrotT = self.cv("rrotT")

        def qk_prep(dst_fn, col0, gain, t0, n, rope):
            ps = self.psb[0 + (self._atp % 2)]; self._atp += 1
            self.proj_fm(ps[:, 0:n], wg, col0, 128, t0, n)
            sq = sqr.next()
            k.act(sq[:, 0:n], ps[:, 0:n], AF.Square)
            pss = self.psb[2]
            k.mm(pss[:, 0:n], bo[:], sq[:, 0:n])
            rs = rsr.next()
            k.act(rs[:, 0:n], pss[:, 0:n], AF.Ln, bias=EPS, scale=1.0 / 64)
            k.act(rs[:, 0:n], rs[:, 0:n], AF.Exp, scale=-0.5)
            kg = kgr.next()
            k.act(kg[:, 0:n], ps[:, 0:n], AF.Copy, scale=gain)
            if rope:
                psr = self.psb[3]
                k.mm(psr[:, 0:n], rrotT, kg[:, 0:n])
                t1 = t1r.next(); t2 = t2r.next()
                k.tt(t1[:, 0:n], kg[:, 0:n], cs[:, 0, t0 - LC:t0 - LC + n], ALU.mult)
                k.tt(t2[:, 0:n], psr[:, 0:n], cs[:, 1, t0 - LC:t0 - LC + n], ALU.mult)
                k.tt(t1[:, 0:n], t1[:, 0:n], t2[:, 0:n], ALU.add)
                k.tt(dst_fn(t0, n), t1[:, 0:n], rs[:, 0:n], ALU.mult)
            else:
                k.tt(dst_fn(t0, n), kg[:, 0:n], rs[:, 0:n], ALU.mult)

        import os
        upto = int(os.environ.get("ATT_UPTO", "9"))
        if upto < 2:
            return
        self._atp = 0
        qn = self.pvv("qn%d" % l)
        kn = self.pvv("kn%d" % l)
        for (t0, n, isctx) in self.tblocks(True):
            qk_prep(lambda a, b: KT[:, a:a + b], 256, kn, t0, n, not isctx)
            if (not isctx) or with_ctx_out:
                for c in range(2):
                    qk_prep(lambda a, b, c=c: QT[:, c, a:a + b], c * 128, qn, t0, n, not isctx)
        if upto < 3:
            return
        for j in range(NTILE):
            ps = self.psb[j % 2]
            for kk in range(8):
                k.mm(ps[:, 0:128], self.hT[:, kk, j * 128:(j + 1) * 128], wg[:, kk, 384:512], start=(kk == 0), stop=(kk == 7))
            vmode = os.environ.get("VMODE", "act")
            for kv in range(2):
                for dup in range(2):
                    if vmode == "none":
                        continue
                    k.copy(Vd[:, j, kv, dup * 64:(dup + 1) * 64], ps[:, kv * 64:(kv + 1) * 64],
                           eng=(("act" if dup else "dve") if vmode == "mix" else vmode))
        if upto < 4:
            return
        qblocks = self.tblocks(with_ctx_out)
        it = 0
        for (t0, n, isctx) in qblocks:
            tiles = [0, 1] if isctx else list(range(NTILE))
            mx = mxr.next()
            for c in range(2):
                for hp in range(2):
                    psO = self.psb[4 + it % 2]
                    psDn = self.psb[6 + it % 2]
                    it += 1
                    rows = slice(hp * 64, (hp + 1) * 64)
                    def s_stage(ji):
                        j = tiles[ji]
                        psS = self.psb[ji % 4]
                        k.mm(psS[:, 0:n], KT[rows, j * 128:(j + 1) * 128], QT[rows, c, t0:t0 + n])
                        pt = ptr.next()
                        k.act(pt[:, 0:n], psS[:, 0:n], AF.Exp, scale=0.125)
                        return pt

                    def pv_stage(ji, pt):
                        j = tiles[ji]
                        k.mm(psO[:, 0:n], Vd[:, j, hp, :], pt[:, 0:n], start=(ji == 0), stop=(ji == len(tiles) - 1))
                        k.mm(psDn[:, 0:n], self.onesb[:], pt[:, 0:n], start=(ji == 0), stop=(ji == len(tiles) - 1))

                    pts = {0: s_stage(0)}
                    for ji in range(len(tiles)):
                        if ji + 1 < len(tiles):
                            pts[ji + 1] = s_stage(ji + 1)
                        pv_stage(ji, pts.pop(ji))
                    rd = rdr.next()
                    k.recip(rd[rows, 0:n], psDn[rows, 0:n])
                    k.tt(mx[rows, c, 0:n], psO[rows, 0:n], rd[rows, 0:n], ALU.mult)
            if upto < 5:
                continue
            self.wout_apply(s, l, wo, 2, 128, mx, t0, n, isctx)

    def moe(self, s, l, with_ctx_out):
        P, k = self.P, self.k
        blocks = self.tblocks(with_ctx_out)
        tiles = list(range(0 if with_ctx_out else 2, NTILE))
        nt = NTILE
        o = OFF_SCR
        wr = P.sb("mo_wr", [128, 8, 20], BF16, o, group="scr"); o = wr.end
        lg = P.sb("mo_lg", [128, nt, 20], F32, o, group="scr"); o = lg.end
        comb = P.sb("mo_comb", [128, nt, 16], F32, o, group="scr"); o = comb.end
        sm = {}
        for nm, w in (("gmax", 1), ("gsh", 4), ("gsum", 1), ("gp", 1), ("ohg", 4), ("prod", 16), ("ing", 4), ("m1", 1),
                      ("oh1", 4), ("ing2", 4), ("m2", 1), ("oh2", 4), ("dl", 1), ("ex", 1), ("den", 1), ("p1", 1),
                      ("w1", 1), ("w2", 1), ("cw", 4), ("cw2", 4)):
            sm[nm] = P.sb("mo_" + nm, [128, nt, w], F32, o, group="scr"); o = sm[nm].end
        wsets = []
        for i in range(2):
            g = P.sb("mo_g%d" % i, [128, 8, 512], BF16, o, group="scr"); o = g.end
            u = P.sb("mo_u%d" % i, [128, 8, 512], BF16, o, group="scr"); o = u.end
            d = P.sb("mo_d%d" % i, [128, 4, 1024], BF16, o, group="scr"); o = d.end
            wsets.append((g, u, d))
        wrot = Rot(wsets)
        bcs = Rot([P.sb("mo_bc%d" % i, [128, 512], F32, o + i * 2048, group="scr") for i in range(2)]); o += 4096
        sgr = Rot([P.sb("mo_sg%d" % i, [128, 512], F32, o + i * 2048, group="scr") for i in range(2)]); o += 4096
        tr = Rot([P.sb("mo_t%d" % i, [128, 512], F32, o + i * 2048, group="scr") for i in range(2)]); o += 4096
        hwr = Rot([P.sb("mo_hw%d" % i, [128, 4, 512], BF16, o + i * 4096, group="scr") for i in range(2)]); o += 8192
        assert o <= SB_END, o
        self.load_w(wr[:], self.rw_d[l])
        def load_expert(e):
            g, u, d = wrot.next()
            self.load_w(g[:], self.wg_d[l, e].rearrange("(k p) n -> p k n", p=128))
            self.load_w(u[:], self.wu_d[l, e].rearrange("(k p) n -> p k n", p=128))
            self.load_w(d[:], self.wd_d[l, e].rearrange("(k p) n -> p k n", p=128))
            return g, u, d
        nxt = load_expert(0)
        ps = self.psrot.next()
        for j in tiles:
            for kk in range(8):
                k.mm(ps[:, j * 20:(j + 1) * 20], self.hT[:, kk, j * 128:(j + 1) * 128], wr[:, kk, :],
                     start=(kk == 0), stop=(kk == 7))
        j0, j1 = tiles[0], tiles[-1] + 1
        njt = j1 - j0
        rb = self.pvv("rb%d" % l)
        psv = ps[:, j0 * 20:j1 * 20]
        k.tt(lg[:, j0:j1, :], psv.w(psv.ap.rearrange("p (j c) -> p j c", c=20)),
             rb.w(rb.ap.unsqueeze(1).broadcast_to([128, njt, 20])), ALU.add)
        S = lambda nm: sm[nm][:, j0:j1, :]
        def bc(v, w):
            return v.w(v.ap.broadcast_to([128, njt, w]))
        gl = lg[:, j0:j1, 0:4]
        k.reduce(S("gmax"), gl, ALU.max)
        k.tt(S("gsh"), gl, bc(S("gmax"), 4), ALU.subtract)
        k.tt(S("ohg"), gl, bc(S("gmax"), 4), ALU.is_equal)
        k.act(S("gsh"), S("gsh"), AF.Exp)
        k.reduce(S("gsum"), S("gsh"), ALU.add)
        k.recip(S("gp"), S("gsum"))
        el = lg[:, j0:j1, 4:20]
        el4 = el.w(el.ap.rearrange("p j (g e) -> p j g e", e=4))
        ohg = S("ohg")
        pr = S("prod")
        pr4 = pr.w(pr.ap.rearrange("p j (g e) -> p j g e", e=4))
        k.tt(pr4, el4, ohg.w(ohg.ap.unsqueeze(3).broadcast_to([128, njt, 4, 4])), ALU.mult)
        k.reduce(S("ing"), pr.w(pr.ap.rearrange("p j (g e) -> p j e g", e=4)), ALU.add)
        k.reduce(S("m1"), S("ing"), ALU.max)
        k.tt(S("oh1"), S("ing"), bc(S("m1"), 4), ALU.is_equal)
        k.stt(S("ing2"), S("oh1"), -1e30, S("ing"), ALU.mult, ALU.add)
        k.reduce(S("m2"), S("ing2"), ALU.max)
        k.tt(S("oh2"), S("ing2"), bc(S("m2"), 4), ALU.is_equal)
        k.tt(S("dl"), S("m2"), S("m1"), ALU.subtract)
        k.act(S("ex"), S("dl"), AF.Exp)
        k.ts(S("den"), S("ex"), 1.0, ALU.add)
        k.recip(S("p1"), S("den"))
        k.tt(S("w1"), S("p1"), S("gp"), ALU.mult)
        k.tt(S("w2"), S("w1"), S("ex"), ALU.mult)
        k.tt(S("cw"), S("oh1"), bc(S("w1"), 4), ALU.mult)
        k.tt(S("cw2"), S("oh2"), bc(S("w2"), 4), ALU.mult)
        k.tt(S("cw"), S("cw"), S("cw2"), ALU.add)
        cb = comb[:, j0:j1, :]
        cw = S("cw")
        k.tt(cb.w(cb.ap.rearrange("p j (g e) -> p j g e", e=4)),
             ohg.w(ohg.ap.unsqueeze(3).broadcast_to([128, njt, 4, 4])),
             cw.w(cw.ap.unsqueeze(2).broadcast_to([128, njt, 4, 4])), ALU.mult)
        if self.debug and self.debug[0] == "comb" and s == 0 and l == 0:
            self.toks_out.append(k.store(self.dbg_d, comb[:]))
        ident = self.cv("ident")
        wmap = {0: nxt}
        if 16 > 1:
            wmap[1] = load_expert(1)
        items = [(e, bi) for e in range(16) for bi in range(len(blocks))]
        hws = {}

        def stage_a(e, bi):
            g, u, d = wmap[e]
            t0, n, isctx = blocks[bi]
            psB = self.psrot.next()
            for jj in range(n // 128):
                j = t0 // 128 + jj
                cv_ = comb[:, j, e:e + 1]
                k.mm(psB[:, jj * 128:(jj + 1) * 128], cv_.w(cv_.ap.broadcast_to([128, 128])), ident)
            bcb = bcs.next()
            k.copy(bcb[:, 0:n], psB[:, 0:n], eng="act")
            hw = hwr.next()
            hws[(e, bi)] = hw
            for fc in range(4):
                psG = self.psrot.next()
                self.proj_fm(psG[:, 0:n], g, fc * 128, 128, t0, n)
                psU = self.psrot.next()
                self.proj_fm(psU[:, 0:n], u, fc * 128, 128, t0, n)
                sg = sgr.next()
                k.act(sg[:, 0:n], psG[:, 0:n], AF.Silu)
                tt_ = tr.next()
                k.tt(tt_[:, 0:n], sg[:, 0:n], psU[:, 0:n], ALU.mult)
                k.tt(hw[:, fc, 0:n], tt_[:, 0:n], bcb[:, 0:n], ALU.mult)

        def stage_b(e, bi):
            g, u, d = wmap[e]
            t0, n, isctx = blocks[bi]
            col = 2 if isctx else s
            hw = hws.pop((e, bi))
            for oc in range(8):
                psD = self.psrot.next()
                for fc in range(4):
                    k.mm(psD[:, 0:n], d[:, fc, oc * 128:(oc + 1) * 128], hw[:, fc, 0:n], start=(fc == 0), stop=(fc == 3))
                self.resid_add(oc, t0, n, psD, self.mod[l][:, 40 + oc, col:col + 1])
            if bi == len(blocks) - 1 and e + 2 < 16:
                wmap[e + 2] = load_expert(e + 2)

        for i, it_ in enumerate(items):
            stage_a(*it_)
            if i > 0:
                stage_b(*items[i - 1])
        stage_b(*items[-1])


def pack_rw(inp):
    rw = np.concatenate([inp["router_group_w"], inp["router_expert_w"]], axis=2)
    return np.ascontiguousarray(rw.reshape(DEPTH, 8, 128, 20).transpose(0, 2, 1, 3), dtype=np.float32)


def pack_rl(inp):
    return np.ascontiguousarray(np.repeat(inp["ret_decay_logit"], 64, axis=-1), dtype=np.float32)


_NC_CACHE = {}


def kernel(**inp):
    inp = {k: np.asarray(v) for k, v in inp.items()}
    ncores = 8
    nseq = 2
    if "nc" not in _NC_CACHE:
        mk = MK(nseq=nseq, nlayers=DEPTH, stages=("hgrn", "conv", "ret", "attn", "moe"))
        _NC_CACHE["nc"] = mk.build()
        _NC_CACHE["mk"] = mk
    nc, mk = _NC_CACHE["nc"], _NC_CACHE["mk"]
    pv = pack_pv(inp, nseq)
    cst = mk.cstl.array()
    rw = pack_rw(inp)
    rope = rope_tables()
    hl = np.ascontiguousarray(inp["hgrn_lb_logits"], dtype=np.float32)
    rl = pack_rl(inp)
    maps = []
    for c in range(ncores):
        b0 = c * nseq
        cc = np.stack([fm(inp["c"][b0]), fm(inp["c"][b0 + 1]), fm(inp["c_ctx"])], axis=2).reshape(128, 24)
        maps.append({
            "x": np.ascontiguousarray(inp["x"][b0:b0 + nseq], dtype=np.float32),
            "ctx": np.ascontiguousarray(inp["ctx"][b0:b0 + nseq], dtype=np.float32),
            "cc": np.ascontiguousarray(cc, dtype=np.float32),
            "pv": pv, "cst": cst, "rope": rope, "hl": hl, "rl": rl, "rw": rw,
            "ada_w": inp["ada_w"], "w_in": inp["w_in"], "w_out": inp["w_out"],
            "wg": inp["expert_w_gate"], "wu": inp["expert_w_up"], "wd": inp["expert_w_down"],
        })
    res = run_bass_kernel_spmd(nc, maps, core_ids=list(range(ncores)))
    out = np.concatenate([np.asarray(r["out"]) for r in res.results], axis=0)
    return out.astype(np.float32)
```

```python
import numpy as np
import concourse.bass as bass
import concourse.mybir as mybir
from concourse.bass_utils import run_bass_kernel_spmd

F32 = mybir.dt.float32
BF16 = mybir.dt.bfloat16
AF = mybir.ActivationFunctionType
ALU = mybir.AluOpType

D = 1024
T = 2048
LC = 256
NT = LC + T
NTILE = NT // 128
DEPTH = 2
INW = 3584
EPS = 1e-6
ESZ = {F32: 4, BF16: 2}


class TInfo:
    def __init__(self, name, space, shape, dtype, handle, off, group):
        self.name, self.space, self.shape, self.dtype, self.h = name, space, tuple(shape), dtype, handle
        self.off = off
        self.group = group
        self.esz = ESZ[dtype]
        st = [1] * len(shape)
        for i in range(len(shape) - 2, 0, -1):
            st[i] = st[i + 1] * shape[i + 1]
        self.strides = st

    def __getitem__(self, idx):
        if not isinstance(idx, tuple):
            idx = (idx,)
        idx = idx + (slice(None),) * (len(self.shape) - len(idx))
        reg = []
        for i, s in zip(idx, self.shape):
            if isinstance(i, int):
                reg.append((i, i + 1))
            else:
                lo = 0 if i.start is None else i.start
                hi = s if i.stop is None else i.stop
                assert 0 <= lo < hi <= s, (self.name, idx)
                reg.append((lo, hi))
        return V(self.h[idx], self, tuple(reg))


class V:
    __slots__ = ("ap", "t", "reg", "blo", "bhi")

    def __init__(self, ap, t, reg):
        self.ap, self.t, self.reg = ap, t, reg
        lo = hi = 0
        for i in range(1, len(reg)):
            lo += reg[i][0] * t.strides[i]
            hi += (reg[i][1] - 1) * t.strides[i]
        self.blo = t.off + lo * t.esz
        self.bhi = t.off + (hi + 1) * t.esz

    def w(self, ap):
        return V(ap, self.t, self.reg)


def _overlap(a, b):
    if a.t.space == "ps":
        return True
    if a.reg[0][0] >= b.reg[0][1] or b.reg[0][0] >= a.reg[0][1]:
        return False
    if a.t is b.t:
        for (l1, h1), (l2, h2) in zip(a.reg[1:], b.reg[1:]):
            if l1 >= h2 or l2 >= h1:
                return False
        return True
    return a.blo < b.bhi and b.blo < a.bhi


def _covers(a, b):
    if a.t.space == "ps":
        return True
    if a.t is b.t:
        for (l1, h1), (l2, h2) in zip(a.reg, b.reg):
            if l1 > l2 or h1 < h2:
                return False
        return True
    return False


class Prog:
    CE = ("pe", "act", "dve", "pool")

    def __init__(self, nc, ndma=8):
        self.nc = nc
        self.ops = {e: [] for e in ("pe", "act", "dve", "pool", "sp")}
        self.cnt = {}
        self.known = {e: {} for e in self.ops}
        self.snap = {}
        self.groups = {}
        self.ndma = ndma
        self.dma_i = {"sp": 0, "pool": 0}
        self.tensors = {}
        self.nops = 0

    def sb(self, name, shape, dtype, off, group="sb"):
        h = self.nc.alloc_sbuf_tensor_at(name, list(shape), dtype, offset=off)
        t = TInfo(name, "sb", shape, dtype, h, off, group)
        nbytes = int(np.prod(shape[1:])) * t.esz
        t.end = (off + nbytes + 31) // 32 * 32
        self.tensors[name] = t
        return t

    def ps(self, name):
        h = self.nc.alloc_psum_tensor(name, [128, 512], F32)
        return TInfo(name, "ps", [128, 512], F32, h, 0, name)

    def dram(self, name, shape, dtype, kind):
        h = self.nc.dram_tensor(name, list(shape), dtype, kind=kind)
        return h

    def _deps(self, eng, reads, writes):
        need = {}

        def add(tok, weng):
            s, v = tok
            if need.get(s, 0) < v:
                need[s] = v

        for r in reads:
            g = self.groups.setdefault(r.t.group, ([], []))
            for (wv, tok, weng) in g[0]:
                if _overlap(wv, r):
                    add(tok, weng)
            if r.t.space == "ps":
                for (rv, tok, reng) in g[1]:
                    if reng != eng:
                        add(tok, reng)
        for w in writes:
            g = self.groups.setdefault(w.t.group, ([], []))
            for (wv, tok, weng) in g[0]:
                if _overlap(wv, w):
                    if not (tok[0] == eng and eng == "pe"):
                        add(tok, weng)
            for (rv, tok, reng) in g[1]:
                if _overlap(rv, w):
                    if not (tok[0] == eng and eng == "pe"):
                        add(tok, reng)
        return need

    def _commit(self, eng, reads, writes, tok):
        for r in reads:
            g = self.groups[r.t.group]
            lst = g[1]
            for i, (rv, t2, e2) in enumerate(lst):
                if t2[0] == tok[0] and rv.t is r.t and rv.reg == r.reg:
                    lst[i] = (r, tok, eng)
                    break
            else:
                lst.append((r, tok, eng))
        for w in writes:
            g = self.groups[w.t.group]
            g0 = [x for x in g[0] if not _covers(w, x[0])]
            g1 = [x for x in g[1] if not _covers(w, x[0])]
            g0.append((w, tok, eng))
            g[0][:] = g0
            g[1][:] = g1

    def _waits(self, eng, need):
        known = self.known[eng]
        waits = []
        for s, v in need.items():
            if known.get(s, 0) < v:
                waits.append((s, v))
        for s, v in waits:
            if known.get(s, 0) < v:
                known[s] = v
            sn = self.snap.get((s, v))
            if sn:
                for k2, v2 in sn.items():
                    if known.get(k2, 0) < v2:
                        known[k2] = v2
        return waits

    def op(self, eng, fn, reads=(), writes=()):
        reads = [r for r in reads if r is not None and r.t.space != "dram"]
        writes = [w for w in writes if w.t.space != "dram"]
        need = self._deps(eng, reads, writes)
        waits = self._waits(eng, need)
        n = self.cnt.get(eng, 0) + 1
        self.cnt[eng] = n
        tok = (eng, n)
        kn = self.known[eng]
        self.snap[tok] = dict(kn)
        self._commit(eng, reads, writes, tok)
        self.ops[eng].append((waits, fn, tok, 1))
        self.nops += 1
        return tok

    def dma(self, q, fn, reads=(), writes=()):
        reads = [r for r in reads if r.t.space != "dram"]
        writes = [w for w in writes if w.t.space != "dram"]
        need = self._deps("dma", reads, writes)
        i = self.dma_i[q]
        self.dma_i[q] = i + 1
        sem = "%s_d%d" % (q, i % self.ndma)
        prev = self.cnt.get(sem, 0)
        if prev:
            if need.get(sem, 0) < prev:
                need[sem] = prev
        waits = self._waits(q, need)
        self.cnt[sem] = prev + 16
        tok = (sem, prev + 16)
        self.snap[tok] = dict(self.known[q])
        self._commit("dma", reads, writes, tok)
        self.ops[q].append((waits, fn, tok, 16))
        self.nops += 1
        return tok

    def wait_all(self, eng, toks):
        need = {}
        for s, v in toks:
            if need.get(s, 0) < v:
                need[s] = v
        waits = self._waits(eng, need)
        if waits:
            self.ops[eng].append((waits, None, None, 0))

    def emit(self):
        nc = self.nc
        names = sorted(self.cnt.keys())
        sems = {}
        import contextlib
        with contextlib.ExitStack() as es:
            for s in names:
                sems[s] = es.enter_context(nc.semaphore("s_" + s))
            block = es.enter_context(nc.Block())

            def run(e, lst):
                for waits, fn, tok, amt in lst:
                    for s, v in waits:
                        e.wait_ge(sems[s], v)
                    if fn is not None:
                        ins = fn(e)
                        ins.then_inc(sems[tok[0]], amt)

            ops = self.ops

            @block.tensor
            def _(e):
                run(e, ops["pe"])

            @block.scalar
            def _(e):
                run(e, ops["act"])

            @block.vector
            def _(e):
                run(e, ops["dve"])

            @block.gpsimd
            def _(e):
                run(e, ops["pool"])

            @block.sync
            def _(e):
                run(e, ops["sp"])


class K:
    def __init__(self, P):
        self.P = P

    def mm(self, out, lhsT, rhs, start=True, stop=True):
        self.P.op("pe", lambda e: e.matmul(out.ap, lhsT.ap, rhs.ap, start=start, stop=stop),
                  reads=[lhsT, rhs], writes=[out])

    def transpose(self, out, in_, ident):
        self.P.op("pe", lambda e: e.transpose(out.ap, in_.ap, ident.ap), reads=[in_, ident], writes=[out])

    def act(self, out, in_, func, bias=None, scale=None, eng="act"):
        kw = {}
        rd = [in_]
        if bias is not None:
            if isinstance(bias, V):
                kw["bias"] = bias.ap
                rd.append(bias)
            else:
                kw["bias"] = bias
        if scale is not None:
            if isinstance(scale, V):
                kw["scale"] = scale.ap
                rd.append(scale)
            else:
                kw["scale"] = scale
        self.P.op("act", lambda e: e.activation(out.ap, in_.ap, func, **kw), reads=rd, writes=[out])

    def tt(self, out, in0, in1, op, eng="dve"):
        self.P.op(eng, lambda e: e.tensor_tensor(out.ap, in0.ap, in1.ap, op), reads=[in0, in1], writes=[out])

    def ts(self, out, in0, s1, op0, s2=None, op1=None, eng="dve"):
        rd = [in0]
        a1 = s1.ap if isinstance(s1, V) else s1
        a2 = s2.ap if isinstance(s2, V) else s2
        if isinstance(s1, V):
            rd.append(s1)
        if isinstance(s2, V):
            rd.append(s2)
        if op1 is None:
            self.P.op(eng, lambda e: e.tensor_scalar(out.ap, in0.ap, a1, None, op0), reads=rd, writes=[out])
        else:
            self.P.op(eng, lambda e: e.tensor_scalar(out.ap, in0.ap, a1, a2, op0, op1), reads=rd, writes=[out])

    def stt(self, out, in0, s, in1, op0, op1):
        rd = [in0, in1]
        a = s.ap if isinstance(s, V) else s
        if isinstance(s, V):
            rd.append(s)
        self.P.op("dve", lambda e: e.scalar_tensor_tensor(out.ap, in0.ap, a, in1.ap, op0, op1), reads=rd, writes=[out])

    def copy(self, out, in_, eng="dve"):
        if eng == "act":
            self.P.op("act", lambda e: e.copy(out.ap, in_.ap), reads=[in_], writes=[out])
        else:
            self.P.op(eng, lambda e: e.tensor_copy(out.ap, in_.ap), reads=[in_], writes=[out])

    def recip(self, out, in_):
        self.P.op("dve", lambda e: e.reciprocal(out.ap, in_.ap), reads=[in_], writes=[out])

    def memset(self, out, val, eng="dve"):
        self.P.op(eng, lambda e: e.memset(out.ap, val), writes=[out])

    def reduce(self, out, in_, op, eng="dve"):
        self.P.op(eng, lambda e: e.tensor_reduce(out.ap, in_.ap, mybir.AxisListType.X, op), reads=[in_], writes=[out])

    def load(self, out, src_ap, q="sp"):
        self.P.dma(q, lambda e: e.dma_start(out=out.ap, in_=src_ap), writes=[out])

    def store(self, dst_ap, in_, q="sp"):
        return self.P.dma(q, lambda e: e.dma_start(out=dst_ap, in_=in_.ap), reads=[in_])


class Pack:
    def __init__(self, rows):
        self.rows = rows
        self.cols = 0
        self.ent = {}
        self.parts = []

    def add(self, name, arr):
        arr = np.ascontiguousarray(arr, dtype=np.float32).reshape(self.rows, -1)
        self.ent[name] = (self.cols, arr.shape[1])
        self.cols += arr.shape[1]
        self.parts.append(arr)

    def array(self):
        return np.ascontiguousarray(np.concatenate(self.parts, axis=1))


def fm(v):
    v = np.asarray(v)
    return v.reshape(-1, 128).T


def hd(v):
    a = np.asarray(v).reshape(4, 64).T
    return np.concatenate([a, a], axis=0)


def pv_layout():
    p = Pack(128)
    z = np.zeros
    for l in range(DEPTH):
        p.add("nmix%d" % l, z((128, 8)))
        p.add("nffn%d" % l, z((128, 8)))
        p.add("adab%d" % l, z((128, 48)))
        p.add("qn%d" % l, z((128, 1)))
        p.add("kn%d" % l, z((128, 1)))
        p.add("hnorm%d" % l, z((128, 4)))
        p.add("rnorm%d" % l, z((128, 4)))
        p.add("convw%d" % l, z((128, 6)))
        p.add("rb%d" % l, z((128, 20)))
        for d in range(2):
            p.add("hlb%d_%d" % (l, d), z((128, 4)))
            p.add("rdl%d_%d" % (l, d), z((128, 4)))
    return p


def pack_pv(inp, nseq):
    p = Pack(128)
    for l in range(DEPTH):
        p.add("nmix%d" % l, fm(inp["norm_mix"][l]))
        p.add("nffn%d" % l, fm(inp["norm_ffn"][l]))
        p.add("adab%d" % l, fm(inp["ada_b"][l]))
        p.add("qn%d" % l, np.tile(inp["q_norm"][l], 2).reshape(128, 1))
        p.add("kn%d" % l, np.tile(inp["k_norm"][l], 2).reshape(128, 1))
        p.add("hnorm%d" % l, hd(inp["hgrn_norm"][l]))
        p.add("rnorm%d" % l, hd(inp["ret_norm"][l]))
        p.add("convw%d" % l, np.stack([fm(inp["conv_w"][l, j]) for j in range(3)], axis=1))
        rb = np.concatenate([inp["router_group_b"][l], inp["router_expert_b"][l]])
        p.add("rb%d" % l, np.broadcast_to(rb[None, :], (128, 20)))
        for d in range(2):
            p.add("hlb%d_%d" % (l, d), hd(inp["hgrn_lb_logits"][l, d]))
            p.add("rdl%d_%d" % (l, d), np.broadcast_to(inp["ret_decay_logit"][l, d][None, :], (128, 4)))
    return p.array()


def rope_tables():
    t = np.arange(T)
    row = (t // 64).astype(np.float32)
    col = (t % 64).astype(np.float32)
    nf = 16
    inv = (np.float32(10000.0) ** (-np.arange(nf, dtype=np.float32) / nf)).astype(np.float32)
    ang = np.concatenate([row[:, None] * inv, col[:, None] * inv], axis=-1)
    cos = np.repeat(np.cos(ang), 2, axis=1).T
    sin = np.repeat(np.sin(ang), 2, axis=1).T
    return np.ascontiguousarray(np.stack([np.tile(cos, (2, 1)), np.tile(sin, (2, 1))]), dtype=np.float32)


def make_consts():
    c = Pack(128)
    c.add("ident", np.eye(128))
    p = np.arange(128)
    c.add("bo64", (p[:, None] // 64 == p[None, :] // 64).astype(np.float32))
    rT = np.zeros((128, 128), np.float32)
    for i in range(64):
        rT[2 * i + 1, 2 * i] = -1.0
        rT[2 * i, 2 * i + 1] = 1.0
    c.add("rrotT", rT)
    sp = p[:, None]
    sc = p[None, :]
    same = (sp // 64 == sc // 64)
    mid = (sc // 64) * 64 + 31
    c.add("M0", same * ((sp <= sc).astype(np.float32) - (sp <= mid).astype(np.float32)))
    c.add("M1", same * ((sp >= sc).astype(np.float32) - (sp >= mid).astype(np.float32)))
    c.add("A0", (same & (sp <= sc)).astype(np.float32))
    c.add("A1", (same & (sp >= sc)).astype(np.float32))
    c.add("D0", (same & (sp > sc)).astype(np.float32))
    c.add("D1", (same & (sp < sc)).astype(np.float32))
    c.add("K0", (same & (sp <= sc)).astype(np.float32))
    c.add("K1", (same & (sp >= sc)).astype(np.float32))
    c.add("Sel", (p[:, None] // 64 == np.arange(2)[None, :]).astype(np.float32))
    return c


SB0 = 16512
OFF_XT = SB0
OFF_HT = OFF_XT + 8 * NT * 4
OFF_CONST = OFF_HT + 8 * NT * 2
CONST_BYTES = 14336
OFF_SCR = OFF_CONST + CONST_BYTES
SB_END = 229344 - 3 * 2048


class Rot:
    def __init__(self, bufs):
        self.bufs, self.i = bufs, 0

    def next(self):
        b = self.bufs[self.i % len(self.bufs)]
        self.i += 1
        return b


class MK:
    def __init__(self, nseq=2, nlayers=2, debug=None, stages=("mix", "moe")):
        self.nseq, self.nlayers, self.debug, self.stages = nseq, nlayers, debug, stages

    def build(self):
        nc = bass.Bass("TRN2", target_bir_lowering=False)
        self.nc = nc
        P = Prog(nc)
        self.P = P
        k = K(P)
        self.k = k
        nseq = self.nseq
        self.pvl = pv_layout()
        self.cstl = make_consts()
        dt = nc.dram_tensor
        self.x_d = dt("x", [nseq, T, D], F32, kind="ExternalInput").ap()
        self.ctx_d = dt("ctx", [nseq, LC, D], F32, kind="ExternalInput").ap()
        self.cc_d = dt("cc", [128, 24], F32, kind="ExternalInput").ap()
        self.pv_d = dt("pv", [128, self.pvl.cols], F32, kind="ExternalInput").ap()
        self.cst_d = dt("cst", [128, self.cstl.cols], F32, kind="ExternalInput").ap()
        self.adaw_d = dt("ada_w", [DEPTH, D, 6 * D], F32, kind="ExternalInput").ap()
        self.win_d = dt("w_in", [DEPTH, D, INW], F32, kind="ExternalInput").ap()
        self.wout_d = dt("w_out", [DEPTH, D, D], F32, kind="ExternalInput").ap()
        self.rope_d = dt("rope", [2, 128, T], F32, kind="ExternalInput").ap()
        self.hl_d = dt("hl", [DEPTH, 2, 256], F32, kind="ExternalInput").ap()
        self.rl_d = dt("rl", [DEPTH, 2, 256], F32, kind="ExternalInput").ap()
        self.rw_d = dt("rw", [DEPTH, 128, 8, 20], F32, kind="ExternalInput").ap()
        self.wg_d = dt("wg", [DEPTH, 16, D, 512], F32, kind="ExternalInput").ap()
        self.wu_d = dt("wu", [DEPTH, 16, D, 512], F32, kind="ExternalInput").ap()
        self.wd_d = dt("wd", [DEPTH, 16, 512, D], F32, kind="ExternalInput").ap()
        self.out_d = dt("out", [nseq, T, D], F32, kind="ExternalOutput").ap()
        if self.debug:
            name, shape, dtype = self.debug
            self.dbg_d = dt("dbg", list(shape), dtype, kind="ExternalOutput").ap()

        self.xT = P.sb("xT", [128, 8, NT], F32, OFF_XT, group="xT")
        self.hT = P.sb("hT", [128, 8, NT], BF16, OFF_HT, group="hT")
        o = OFF_CONST
        self.pv = P.sb("pv", [128, self.pvl.cols], F32, o, group="pv"); o = self.pv.end
        self.cst = P.sb("cst", [128, self.cstl.cols], F32, o, group="cst"); o = self.cst.end
        self.onesb = P.sb("onesb", [128, 128], BF16, o, group="onesb"); o = self.onesb.end
        self.scc = P.sb("scc", [128, 8, 3], F32, o, group="scc"); o = self.scc.end
        self.mod = [P.sb("mod%d" % l, [128, 48, 3], F32, o + l * 576, group="mod%d" % l) for l in range(DEPTH)]
        o += 2 * 576
        self.gpm = [P.sb("gpm%d" % l, [128, 8, 3], F32, o + l * 96, group="gpm%d" % l) for l in range(DEPTH)]
        o += 2 * 96
        self.gpf = [P.sb("gpf%d" % l, [128, 8, 3], F32, o + l * 96, group="gpf%d" % l) for l in range(DEPTH)]
        o += 2 * 96
        assert o <= OFF_CONST + CONST_BYTES, o - OFF_CONST
        self.const_end = o
        self.psb = [P.ps("psb%d" % i) for i in range(8)]
        self.psrot = Rot(self.psb)
        self.toks_out = []

        self.setup()
        for s in range(nseq):
            self.load_x(s)
            for l in range(self.nlayers):
                self.layer(s, l)
            self.store_x(s)
        P.wait_all("sp", self.toks_out)
        P.emit()
        return nc

    def pvv(self, name):
        o, n = self.pvl.ent[name]
        return self.pv[:, o:o + n]

    def pvr(self, name, rows):
        o, n = self.pvl.ent[name]
        return self.pv[0:rows, o:o + n]

    def cv(self, name):
        o, n = self.cstl.ent[name]
        return self.cst[:, o:o + n]

    def setup(self):
        P, k = self.P, self.k
        k.load(self.pv[:], self.pv_d)
        k.load(self.cst[:], self.cst_d)
        k.memset(self.onesb[:], 1.0)
        cc = P.sb("cc_t", [128, 8, 3], F32, OFF_SCR, group="scr")
        k.load(cc[:], self.cc_d.rearrange("p (k c) -> p k c", c=3))
        k.act(self.scc[:], cc[:], AF.Silu)
        NB = 768
        blks = [P.sb("adaw%d" % i, [128, 8, NB], F32, OFF_SCR + 1024 + i * 8 * NB * 4, group="scr") for i in range(2)]
        rot = Rot(blks)
        for l in range(self.nlayers):
            ps = self.psb[l]
            for b in range(6 * D // NB):
                blk = rot.next()
                src = self.adaw_d[l].rearrange("(k p) n -> p k n", p=128)[:, :, b * NB:(b + 1) * NB]
                k.load(blk[:], src)
                for jj in range(NB // 128):
                    j = b * (NB // 128) + jj
                    for kk in range(8):
                        k.mm(ps[:, j * 3:j * 3 + 3], blk[:, kk, jj * 128:(jj + 1) * 128], self.scc[:, kk, :],
                             start=(kk == 0), stop=(kk == 7))
            pv3 = ps[:, 0:144]
            ab = self.pvv("adab%d" % l)
            k.tt(self.mod[l][:], pv3.w(pv3.ap.rearrange("p (j c) -> p j c", c=3)),
                 ab.w(ab.ap.unsqueeze(2).broadcast_to([128, 48, 3])), ALU.add)
            for (gp, nm, m0) in ((self.gpm[l], "nmix%d" % l, 8), (self.gpf[l], "nffn%d" % l, 32)):
                g = self.pvv(nm)
                k.stt(gp[:], self.mod[l][:, m0:m0 + 8, :], 1.0, g.w(g.ap.unsqueeze(2).broadcast_to([128, 8, 3])),
                      ALU.add, ALU.mult)

    def load_x(self, s):
        P, k = self.P, self.k
        bufs = [P.sb("xtm%d" % i, [128, D], F32, OFF_SCR + i * 4096, group="scr") for i in range(3)]
        rot = Rot(bufs)
        ident = self.cv("ident")
        for j in range(NTILE):
            b = rot.next()
            if j < 2:
                src = self.ctx_d[s, j * 128:(j + 1) * 128, :]
            else:
                src = self.x_d[s, (j - 2) * 128:(j - 1) * 128, :]
            k.load(b[:], src)
            for half in range(2):
                ps = self.psrot.next()
                for c in range(4):
                    kk = half * 4 + c
                    k.transpose(ps[:, c * 128:(c + 1) * 128], b[:, kk * 128:(kk + 1) * 128], ident)
                dst = self.xT[:, half * 4:half * 4 + 4, j * 128:(j + 1) * 128]
                srcv = ps[:]
                k.copy(dst, srcv.w(srcv.ap.rearrange("p (c t) -> p c t", c=4)), eng=("act" if half else "dve"))

    def store_x(self, s):
        P, k = self.P, self.k
        bufs = [P.sb("xo%d" % i, [128, D], F32, OFF_SCR + i * 4096, group="scr") for i in range(3)]
        rot = Rot(bufs)
        ident = self.cv("ident")
        for j in range(2, NTILE):
            b = rot.next()
            for half in range(2):
                ps = self.psrot.next()
                for c in range(4):
                    kk = half * 4 + c
                    k.transpose(ps[:, c * 128:(c + 1) * 128], self.xT[:, kk, j * 128:(j + 1) * 128], ident)
                k.copy(b[:, half * 512:(half + 1) * 512], ps[:], eng=("act" if half else "dve"))
            tok = k.store(self.out_d[s, (j - 2) * 128:(j - 1) * 128, :], b[:])
            self.toks_out.append(tok)

    def tblocks(self, with_ctx=True):
        bl = []
        if with_ctx:
            bl.append((0, LC, True))
        for i in range(T // 512):
            bl.append((LC + i * 512, 512, False))
        return bl

    def norm_mod(self, s, l, which, scr_off):
        P, k = self.P, self.k
        gp = (self.gpm if which == 0 else self.gpf)[l]
        sh0 = 0 if which == 0 else 24
        o = scr_off
        sq = Rot([P.sb("nsq%d" % i, [128, 512], BF16, o + i * 1024, group="scr") for i in range(3)])
        o += 3 * 1024
        lnv = P.sb("nln", [128, 512], F32, o, group="scr"); o += 2048
        rstd = Rot([P.sb("nrstd%d" % i, [128, 512], F32, o + i * 2048, group="scr") for i in range(2)])
        o += 2 * 2048
        tmp = Rot([P.sb("ntmp%d" % i, [128, 512], F32, o + i * 2048, group="scr") for i in range(3)])
        o += 3 * 2048
        with_ctx = True
        for (t0, n, isctx) in self.tblocks(with_ctx):
            col = 2 if isctx else s
            ps = self.psrot.next()
            for kk in range(8):
                q = sq.next()
                k.act(q[:, 0:n], self.xT[:, kk, t0:t0 + n], AF.Square)
                k.mm(ps[:, 0:n], self.onesb[:], q[:, 0:n], start=(kk == 0), stop=(kk == 7))
            k.act(lnv[:, 0:n], ps[:, 0:n], AF.Ln, bias=EPS, scale=1.0 / D)
            r = rstd.next()
            k.act(r[:, 0:n], lnv[:, 0:n], AF.Exp, scale=-0.5)
            for kk in range(8):
                t = tmp.next()
                k.stt(t[:, 0:n], self.xT[:, kk, t0:t0 + n], gp[:, kk, col:col + 1], r[:, 0:n], ALU.mult, ALU.mult)
                k.act(self.hT[:, kk, t0:t0 + n], t[:, 0:n], AF.Identity, bias=self.mod[l][:, sh0 + kk, col:col + 1])
        return o

    def mark(self, label):
        if not hasattr(self, "marks"):
            self.marks = []
        self.marks.append((label, dict(self.P.cnt)))

    def layer(self, s, l):
        with_ctx_out = l < DEPTH - 1
        self.mark("s%d l%d norm1" % (s, l))
        o = self.norm_mod(s, l, 0, OFF_SCR)
        if self.debug and self.debug[0] == "hT" and s == 0 and l == 0:
            tok = self.k.store(self.dbg_d, self.hT[:])
            self.toks_out.append(tok)
        if "conv" in self.stages:
            self.mark("s%d l%d conv" % (s, l))
            self.conv_group(s, l, with_ctx_out)
        if "hgrn" in self.stages:
            self.mark("s%d l%d hgrn" % (s, l))
            self.scan_group(s, l, with_ctx_out, "hgrn")
        if "ret" in self.stages:
            self.mark("s%d l%d ret" % (s, l))
            self.scan_group(s, l, with_ctx_out, "ret")
        if "attn" in self.stages:
            self.mark("s%d l%d attn" % (s, l))
            self.attn_group(s, l, with_ctx_out)
        if "moe" in self.stages:
            self.mark("s%d l%d norm2" % (s, l))
            self.norm_mod(s, l, 1, OFF_SCR)
            self.mark("s%d l%d moe" % (s, l))
            self.moe(s, l, with_ctx_out)
        self.mark("s%d l%d end" % (s, l))

    def load_w(self, dst, src_ap):
        self.P.dma("pool", lambda e: e.dma_start(out=dst.ap, in_=src_ap), writes=[dst])

    def proj_fm(self, out, wg, col0, m, t0, n):
        k = self.k
        for kk in range(8):
            k.mm(out, wg[:, kk, col0:col0 + m], self.hT[:, kk, t0:t0 + n], start=(kk == 0), stop=(kk == 7))

    def wout_apply(self, s, l, wo, nk, kp, mix, t0, n, isctx):
        k = self.k
        col = 2 if isctx else s
        for oc in range(8):
            ps = self.psrot.next()
            for c in range(nk):
                k.mm(ps[:, 0:n], wo[0:kp, c, oc * 128:(oc + 1) * 128], mix[0:kp, c, 0:n], start=(c == 0), stop=(c == nk - 1))
            if getattr(self, "conv_upto", 9) < 7:
                continue
            self.resid_add(oc, t0, n, ps, self.mod[l][:, 16 + oc, col:col + 1])

    def resid_add(self, oc, t0, n, ps, gate):
        k = self.k
        if not hasattr(self, "ra_rot"):
            self.ra_rot = Rot([self.P.sb("ra%d" % i, [128, 512], F32, 229344 - (i + 1) * 2048, group="scr") for i in range(3)])
        t = self.ra_rot.next()
        k.act(t[:, 0:n], ps[:, 0:n], AF.Copy, scale=gate)
        k.tt(self.xT[:, oc, t0:t0 + n], self.xT[:, oc, t0:t0 + n], t[:, 0:n], ALU.add)

    def conv_group(self, s, l, with_ctx_out):
        P, k = self.P, self.k
        o = OFF_SCR
        wg = P.sb("cv_wg", [128, 8, 768], BF16, o, group="scr"); o = wg.end
        wo = P.sb("cv_wo", [128, 2, 1024], BF16, o, group="scr"); o = wo.end
        uT = P.sb("cv_u", [128, 2, NT], F32, o, group="scr"); o = uT.end
        tmpc = Rot([P.sb("cv_tc%d" % i, [128, 512], F32, o + i * 2048, group="scr") for i in range(2)]); o += 4096
        acc = Rot([P.sb("cv_acc%d" % i, [128, 512], F32, o + i * 2048, group="scr") for i in range(2)]); o += 4096
        mixb = Rot([P.sb("cv_mix%d" % i, [128, 2, 512], BF16, o + i * 2048, group="scr") for i in range(2)]); o += 4096
        assert o <= SB_END
        self.load_w(wg[:], self.win_d[l].rearrange("(k p) n -> p k n", p=128)[:, :, 1280:2048])
        self.load_w(wo[:], self.wout_d[l][256:512, :].rearrange("(c p) n -> p c n", p=128))
        blocks = self.tblocks(with_ctx_out)
        upto = getattr(self, "conv_upto", 9)
        if upto < 2:
            return
        for (t0, n, isctx) in blocks:
            for c in range(2):
                psC = self.psrot.next()
                self.proj_fm(psC[:, 0:n], wg, 256 + c * 128, 128, t0, n)
                psH = self.psrot.next()
                self.proj_fm(psH[:, 0:n], wg, 512 + c * 128, 128, t0, n)
                tc_ = tmpc.next()
                k.copy(tc_[:, 0:n], psC[:, 0:n], eng="act")
                k.tt(uT[:, c, t0:t0 + n], tc_[:, 0:n], psH[:, 0:n], ALU.mult)
        if upto < 3:
            return
        cw = self.pvv("convw%d" % l)
        co = self.pvl.ent["convw%d" % l][0]
        for (t0, n, isctx) in blocks:
            r0, r1 = (0, LC) if isctx else (LC, NT)
            mb = mixb.next()
            for c in range(2):
                psB = self.psrot.next()
                self.proj_fm(psB[:, 0:n], wg, c * 128, 128, t0, n)
                a = acc.next()
                w0 = self.pv[:, co + 0 + c:co + 1 + c]
                w1 = self.pv[:, co + 2 + c:co + 3 + c]
                w2 = self.pv[:, co + 4 + c:co + 5 + c]
                k.ts(a[:, 0:n], uT[:, c, t0:t0 + n], w1, ALU.mult)
                if upto < 4:
                    continue
                lo = max(t0, r0 + 1)
                k.stt(a[:, lo - t0:n], uT[:, c, lo - 1:t0 + n - 1], w0, a[:, lo - t0:n], ALU.mult, ALU.add)
                hi = min(t0 + n, r1 - 1)
                k.stt(a[:, 0:hi - t0], uT[:, c, t0 + 1:hi + 1], w2, a[:, 0:hi - t0], ALU.mult, ALU.add)
                if upto < 5:
                    continue
                k.tt(mb[:, c, 0:n], a[:, 0:n], psB[:, 0:n], ALU.mult)
            if upto < 6:
                continue
            self.wout_apply(s, l, wo, 2, 128, mb, t0, n, isctx)

    def scan_group(self, s, l, with_ctx_out, kind):
        P, k = self.P, self.k
        hg = kind == "hgrn"
        ncol = 1280 if hg else 1024
        c0 = 0 if hg else 2048
        row0 = 0 if hg else 512
        QC, GC = 0, (1024 if hg else 768)
        VC = 256 if hg else 512
        o = OFF_SCR
        wg = P.sb("sc_wg", [128, 8, ncol], BF16, o, group="scr"); o = wg.end
        wo = P.sb("sc_wo", [64, 4, 1024], BF16, o, group="scr"); o = wo.end
        oacc = P.sb("sc_oacc", [64, 4, NT], BF16, o, group="scr"); o = oacc.end
        def T_(nm, shape, dt_):
            nonlocal o
            t = P.sb("sc_" + nm, list(shape), dt_, o, group="scr"); o = t.end
            return t
        lbt = [T_("lbt%d" % d, (128, 256), F32) for d in range(2)]
        omt = [T_("omt%d" % d, (128, 256), F32) for d in range(2)] if hg else None
        lbf = T_("lbf", (64, 2, 4), F32)
        omf = T_("omf", (64, 2, 4), F32)
        zt = T_("zt", (128, 256), F32); lf = T_("lf", (128, 256), F32)
        kdec = T_("kdec", (128, 256), BF16); vt = T_("vt", (128, 256), BF16)
        eq = T_("eq", (64, 512), F32); ek = T_("ek", (64, 512), F32); ea = T_("ea", (64, 512), F32)
        ed = T_("ed", (128, 256), F32); abv = T_("abv", (64, 4, 2), F32)
        kTf = T_("kTf", (64, 512), F32)
        qTf = None if hg else T_("qTf", (64, 512), F32)
        qtil = T_("qtil", (64, 4, 128), BF16); ktil = T_("ktil", (64, 4, 128), BF16); qabs = T_("qabs", (64, 4, 128), BF16)
        scm = T_("scm", (128, 4, 128), BF16)
        sct = T_("sct", (128, 4, 64), F32)
        kt = P.sb("sc_kt", [128, 256], F32, sct.off, group="scr")
        S = T_("S", (64, 4, 64), F32); Sb = T_("Sb", (64, 4, 64), BF16)
        osum = P.sb("sc_osum", [64, 512], F32, (ea.off if hg else qTf.off), group="scr")
        sg = P.sb("sc_sg", [64, 512], F32, kTf.off, group="scr")
        if hg:
            rsd = P.sb("sc_rsd", [64, 512], F32, eq.off, group="scr")
            sqb = P.sb("sc_sqb", [64, 512], BF16, ek.off, group="scr")
        else:
            sqb = T_("sqb", (64, 512), BF16); rsd = T_("rsd", (64, 512), F32)
        tbuf = T_("tbuf", (64, 4, 512), BF16)
        Sab = T_("Sab", (64, 4, 64), F32)
        ones64 = T_("ones64", (64, 64), BF16)
        assert o <= SB_END, o
        win = self.win_d[l].rearrange("(k p) n -> p k n", p=128)
        self.load_w(wg[:], win[:, :, c0:c0 + ncol])
        self.load_w(wo[:], self.wout_d[l][row0:row0 + 256, :].rearrange("(h d) n -> d h n", d=64))
        k.memset(ones64[:], 1.0)
        k.memset(scm[:], 0.0)
        b = self.psb
        gain = self.pvv(("hnorm%d" if hg else "rnorm%d") % l)
        for d in range(2):
            if hg:
                if l == 0:
                    k.memset(lbt[d][:], 0.0)
                    k.memset(lbf[:, d, :], 0.0)
                else:
                    k.load(lbt[d][:], self.hl_d[1, d:d + 1, :].partition_broadcast(128).rearrange("p a n -> p (a n)"))
                    k.load(zt[:], self.hl_d[0, d:d + 1, :].partition_broadcast(128).rearrange("p a n -> p (a n)"))
                    k.tt(lbt[d][:], lbt[d][:], zt[:], ALU.subtract)
                    k.act(lbt[d][:], lbt[d][:], AF.Sigmoid)
                    k.tt(lbf[:, d, :], self.pvr("hlb1_%d" % d, 64), self.pvr("hlb0_%d" % d, 64), ALU.subtract)
                    k.act(lbf[:, d, :], lbf[:, d, :], AF.Sigmoid)
                k.ts(omt[d][:], lbt[d][:], -1.0, ALU.mult, 1.0, ALU.add)
                k.ts(omf[:, d, :], lbf[:, d, :], -1.0, ALU.mult, 1.0, ALU.add)
            else:
                k.load(lbt[d][:], self.rl_d[l, d:d + 1, :].partition_broadcast(128).rearrange("p a n -> p (a n)"))
                k.act(lbt[d][:], lbt[d][:], AF.Exp, scale=-1.0)
                k.act(lbt[d][:], lbt[d][:], AF.Ln, bias=1.0)
                k.ts(lbt[d][:], lbt[d][:], -1.0, ALU.mult)

        import os
        hup = int(os.environ.get("HG_UPTO", "9"))
        if hup < 2:
            return

        cb = {}
        for nm in ("M0", "M1", "A0", "A1", "D0", "D1"):
            cb[nm] = T_("cb_" + nm, (128, 128), BF16)
            k.copy(cb[nm][:], self.cv(nm))
        cb["Sel"] = T_("cb_Sel", (128, 2), BF16)
        k.copy(cb["Sel"][:], self.cv("Sel"))
        lfh = T_("lfh", (128, 256), BF16)
        lfl = T_("lfl", (128, 256), BF16)
        lfr = zt
        kdec2 = T_("kdec2", (128, 256), BF16); vt2 = T_("vt2", (128, 256), BF16)
        qabs2 = T_("qabs2", (64, 4, 128), BF16); scm2 = T_("scm2", (128, 4, 128), BF16)
        abv2 = T_("abv2", (64, 4, 2), F32) if hg else abv
        sets = [(kdec, vt, qabs, scm, abv), (kdec2, vt2, qabs2, scm2, abv2)]
        k.memset(scm2[:], 0.0)
        assert o <= SB_END, o

        def decay_prep(d, lfv, abv, have_hi=False):
            M, A, Dm, Sel = cb["M%d" % d], cb["A%d" % d], cb["D%d" % d], cb["Sel"]
            if not have_hi:
                k.copy(lfh[:], lfv, eng="act")
            k.tt(lfl[:], lfv, lfh[:], ALU.subtract)
            for i, part in enumerate((lfh, lfl)):
                k.mm(b[3][:, 0:256], Dm[:], part[:], start=(i == 0), stop=(i == 1))
            for h in range(4):
                hc = slice(h * 64, (h + 1) * 64)
                for i, part in enumerate((lfh, lfl)):
                    k.mm(b[4][0:64, h * 128:(h + 1) * 128], part[:, hc], M[:], start=(i == 0), stop=(i == 1))
                for i, part in enumerate((lfh, lfl)):
                    k.mm(b[5][0:64, h * 128:(h + 1) * 128], part[:, hc], A[:], start=(i == 0), stop=(i == 1))
            for h in range(4):
                hc = slice(h * 64, (h + 1) * 64)
                for i, part in enumerate((lfh, lfl)):
                    k.mm(b[3][0:64, 256 + h * 2:258 + h * 2], part[:, hc], Sel[:], start=(i == 0), stop=(i == 1))
            k.act(ed[:], b[3][:, 0:256], AF.Exp)
            k.act(eq[:], b[4][0:64, :], AF.Exp)
            k.act(ek[:], b[4][0:64, :], AF.Exp, scale=-1.0)
            k.act(ea[:], b[5][0:64, :], AF.Exp)
            av = b[3][0:64, 256:264]
            k.act(abv[:], av.w(av.ap.rearrange("p (h u) -> p h u", u=2)), AF.Exp)

        def v3(t):
            return t.w(t.ap.rearrange("p (h t) -> p h t", h=4))

        for d in range(2):
            order = list(range(NTILE)) if d == 0 else [1, 0] + list(range(NTILE - 1, 1, -1))
            uo = (0, 1) if d == 0 else (1, 0)
            k.memset(S[:], 0.0)
            k.memset(Sb[:], 0.0)
            mask = self.cv("K%d" % d)
            if not hg:
                decay_prep(d, lbt[d][:], abv)
                k.ts(eq[:], eq[:], 0.125, ALU.mult)
                k.ts(ea[:], ea[:], 0.125, ALU.mult)
            def front_a(j, p):
                kdec, vt, qabs, scm, abv_p = sets[p]
                t0 = j * 128
                kcol = (512 + d * 256) if hg else 256
                for kk in range(8):
                    k.mm(b[2][:, 0:256], self.hT[:, kk, t0:t0 + 128], wg[:, kk, kcol:kcol + 256], start=(kk == 0), stop=(kk == 7))
                for kk in range(8):
                    k.mm(b[2][:, 256:512], self.hT[:, kk, t0:t0 + 128], wg[:, kk, VC:VC + 256], start=(kk == 0), stop=(kk == 7))
                if hg:
                    k.act(zt[:], b[2][:, 0:256], AF.Sigmoid)
                    k.act(kt[:], b[2][:, 0:256], AF.Sigmoid, scale=-1.0)
                    k.copy(vt[:], b[2][:, 256:512], eng="act")
                    if l > 0:
                        k.tt(zt[:], zt[:], omt[d][:], ALU.mult)
                        k.tt(zt[:], zt[:], lbt[d][:], ALU.add)
                        k.tt(kt[:], kt[:], omt[d][:], ALU.mult)
                    k.act(lf[:], zt[:], AF.Ln)
                    k.act(lfh[:], zt[:], AF.Ln)
                else:
                    k.copy(vt[:], b[2][:, 256:512], eng="act")
                    k.tt(kdec[:], b[2][:, 0:256], ed[:], ALU.mult)

            def front_b(j, p):
                kdec, vt, qabs, scm, abv_p = sets[p]
                isctx = j < 2
                need_out = (not isctx) or with_ctx_out
                t0 = j * 128
                kcol = (512 + d * 256) if hg else 256
                if hg:
                    decay_prep(d, lf[:], abv_p, have_hi=True)
                    k.tt(kdec[:], kt[:], ed[:], ALU.mult)
                if not need_out:
                    return
                for h in range(4):
                    for kk in range(8):
                        k.mm(b[0][0:64, h * 128:(h + 1) * 128], wg[:, kk, QC + h * 64:QC + (h + 1) * 64],
                             self.hT[:, kk, t0:t0 + 128], start=(kk == 0), stop=(kk == 7))
                for h in range(4):
                    for kk in range(8):
                        k.mm(b[1][0:64, h * 128:(h + 1) * 128], wg[:, kk, kcol + h * 64:kcol + (h + 1) * 64],
                             self.hT[:, kk, t0:t0 + 128], start=(kk == 0), stop=(kk == 7))
                if hg:
                    k.act(kTf[:], b[1][0:64, :], AF.Sigmoid, scale=-1.0)
                    if l > 0:
                        omb = omf[:, d, :]
                        k.tt(v3(kTf[:]), v3(kTf[:]), omb.w(omb.ap.unsqueeze(2).broadcast_to([64, 4, 128])), ALU.mult)
                    ksrc = kTf[:]
                else:
                    ksrc = b[1][0:64, :]
                qsrc = b[0][0:64, :]
                k.tt(qtil[:].w(qtil[:].ap.rearrange("p h t -> p (h t)")), qsrc, eq[:], ALU.mult)
                k.tt(qabs[:].w(qabs[:].ap.rearrange("p h t -> p (h t)")), qsrc, ea[:], ALU.mult)
                k.tt(ktil[:].w(ktil[:].ap.rearrange("p h t -> p (h t)")), ksrc, ek[:], ALU.mult)
                mo = self.cstl.ent["K%d" % d][0]
                for u in range(2):
                    us = slice(u * 64, (u + 1) * 64)
                    for h in range(4):
                        k.mm(b[6][us, h * 128 + u * 64:h * 128 + (u + 1) * 64], ktil[:, h, us], qtil[:, h, us])
                for u in range(2):
                    us = slice(u * 64, (u + 1) * 64)
                    sv = b[6][us, :]
                    sv = sv.w(sv.ap.rearrange("p (h t) -> p h t", h=4)[:, :, u * 64:(u + 1) * 64])
                    mk_ = self.cst[us, mo + u * 64:mo + (u + 1) * 64]
                    mkb = mk_.w(mk_.ap.unsqueeze(1).broadcast_to([64, 4, 64]))
                    if hg:
                        k.ts(sct[us, :, :], sv, 1e30, ALU.min, -1e30, ALU.max)
                        k.tt(scm[us, :, us], sct[us, :, :], mkb, ALU.mult)
                    else:
                        k.tt(scm[us, :, us], sv, mkb, ALU.mult)

            def back(j, p):
                kdec, vt, qabs, scm, abv_p = sets[p]
                isctx = j < 2
                need_out = (not isctx) or with_ctx_out
                t0 = j * 128
                for ui, u in enumerate(uo):
                    us = slice(u * 64, (u + 1) * 64)
                    for h in range(4):
                        k.mm(b[5 - ui][0:64, h * 64:(h + 1) * 64], kdec[us, h * 64:(h + 1) * 64],
                             vt[us, h * 64:(h + 1) * 64])
                for ui, u in enumerate(uo):
                    us = slice(u * 64, (u + 1) * 64)
                    if need_out:
                        for h in range(4):
                            oreg = b[7][0:64, h * 128 + u * 64:h * 128 + (u + 1) * 64]
                            k.mm(oreg, vt[:, h * 64:(h + 1) * 64], scm[:, h, us], start=True, stop=False)
                            k.mm(oreg, Sb[:, h, :], qabs[:, h, us], start=False, stop=True)
                    ab = abv_p[:, :, u:u + 1]
                    kvv = b[5 - ui][0:64, 0:256]
                    kvv = kvv.w(kvv.ap.rearrange("p (h e) -> p h e", h=4))
                    k.tt(Sab[:], S[:], ab.w(ab.ap.broadcast_to([64, 4, 64])), ALU.mult)
                    k.tt(Sb[:], Sab[:], kvv, ALU.add)
                    k.tt(S[:], Sab[:], kvv, ALU.add)
                if not need_out:
                    return
                o7 = b[7][0:64, :]
                if d == 0:
                    k.copy(oacc[:, :, t0:t0 + 128], o7.w(o7.ap.rearrange("p (h t) -> p h t", h=4)), eng="act")
                    return
                if isctx:
                    blk0, nblk = 0, LC
                else:
                    blk0 = LC + ((t0 - LC) // 512) * 512
                    nblk = 512
                off = t0 - blk0
                k.tt(v3(osum[:]), o7.w(o7.ap.rearrange("p (h t) -> p h t", h=4)), oacc[:, :, t0:t0 + 128], ALU.add)
                k.act(sqb[:], osum[:], AF.Square)
                k.mm(b[0][0:64, :], ones64[:], sqb[:])
                k.act(rsd[:], b[0][0:64, :], AF.Ln, bias=EPS, scale=1.0 / 64)
                k.act(rsd[:], rsd[:], AF.Exp, scale=-0.5)
                k.tt(osum[:], osum[:], rsd[:], ALU.mult)
                g64 = self.pvr(("hnorm%d" if hg else "rnorm%d") % l, 64)
                k.tt(tbuf[:, :, off:off + 128], v3(osum[:]), g64.w(g64.ap.unsqueeze(2).broadcast_to([64, 4, 128])), ALU.mult)
                if t0 != blk0:
                    return
                for h in range(4):
                    ps = b[h % 2]
                    for kk in range(8):
                        k.mm(ps[0:64, 0:nblk], wg[:, kk, GC + h * 64:GC + (h + 1) * 64],
                             self.hT[:, kk, blk0:blk0 + nblk], start=(kk == 0), stop=(kk == 7))
                    k.act(sg[:, 0:nblk], ps[0:64, 0:nblk], AF.Silu)
                    k.tt(tbuf[:, h, 0:nblk], tbuf[:, h, 0:nblk], sg[:, 0:nblk], ALU.mult)
                col = 2 if isctx else s
                for oc in range(8):
                    ps = b[2 + oc % 2]
                    for h in range(4):
                        k.mm(ps[:, 0:nblk], wo[:, h, oc * 128:(oc + 1) * 128], tbuf[:, h, 0:nblk], start=(h == 0), stop=(h == 3))
                    self.resid_add(oc, blk0, nblk, ps, self.mod[l][:, 16 + oc, col:col + 1])

            front_a(order[0], 0)
            front_b(order[0], 0)
            for i, j in enumerate(order):
                if i + 1 < len(order):
                    front_a(order[i + 1], (i + 1) % 2)
                back(j, i % 2)
                if i + 1 < len(order):
                    front_b(order[i + 1], (i + 1) % 2)

    def _kv_view(self, b, need_out):
        v = b[5][0:64, 0:256] if not need_out else b[3][0:64, 272:528]
        return v.w(v.ap.rearrange("p (h e) -> p h e", h=4))

    def attn_group(self, s, l, with_ctx_out):
        P, k = self.P, self.k
        o = OFF_SCR
        wg = P.sb("at_wg", [128, 8, 512], BF16, o, group="scr"); o = wg.end
        wo = P.sb("at_wo", [128, 2, 1024], BF16, o, group="scr"); o = wo.end
        QT = P.sb("at_QT", [128, 2, NT], BF16, o, group="scr"); o = QT.end
        KT = P.sb("at_KT", [128, NT], BF16, o, group="scr"); o = KT.end
        Vd = P.sb("at_V", [128, NTILE, 2, 128], BF16, o, group="scr"); o = Vd.end
        cs = P.sb("at_cs", [128, 2, T], F32, o, group="scr"); o = cs.end
        bo = P.sb("at_bo", [128, 128], BF16, o, group="scr"); o = bo.end
        def rot(nm, dt_, nb, shape=(128, 512)):
            nonlocal o
            sz = int(np.prod(shape[1:])) * ESZ[dt_]
            r = Rot([P.sb("at_%s%d" % (nm, i), list(shape), dt_, o + i * sz, group="scr") for i in range(nb)])
            o += nb * sz
            return r
        sqr = rot("sq", BF16, 2); rsr = rot("rs", F32, 2); kgr = rot("kg", F32, 2)
        t1r = rot("t1", F32, 2); t2r = rot("t2", F32, 2); ptr = rot("pt", BF16, 3)
        rdr = rot("rd", F32, 2); mxr = rot("mx", BF16, 2, (128, 2, 512))
        assert o <= SB_END, o
        win = self.win_d[l].rearrange("(k p) n -> p k n", p=128)
        for i, h in enumerate((0, 2, 1, 3)):
            self.load_w(wg[:, :, i * 64:(i + 1) * 64], win[:, :, 3072 + h * 64:3072 + (h + 1) * 64])
            c, hp = i // 2, i % 2
            self.load_w(wo[hp * 64:(hp + 1) * 64, c, :], self.wout_d[l][768 + h * 64:768 + (h + 1) * 64, :])
        self.load_w(wg[:, :, 256:512], win[:, :, 3328:3584])
        k.load(cs[:], self.rope_d.rearrange("c p t -> p c t"))
        k.copy(bo[:], self.cv("bo64"))
        rrotT = self.cv("rrotT")

        def qk_stage1(col0, gain, t0, n):
            ps = self.psb[0 + (self._atp % 2)]; self._atp += 1
            self.proj_fm(ps[:, 0:n], wg, col0, 128, t0, n)
            sq = sqr.next()
            k.act(sq[:, 0:n], ps[:, 0:n], AF.Square)
            kg = kgr.next()
            k.act(kg[:, 0:n], ps[:, 0:n], AF.Copy, scale=gain)
            return sq, kg

        def qk_stage2(st, dst_fn, t0, n, rope):
            sq, kg = st
            pss = self.psb[2]
            k.mm(pss[:, 0:n], bo[:], sq[:, 0:n])
            rs = rsr.next()
            k.act(rs[:, 0:n], pss[:, 0:n], AF.Ln, bias=EPS, scale=1.0 / 64)
            k.act(rs[:, 0:n], rs[:, 0:n], AF.Exp, scale=-0.5)
            if rope:
                psr = self.psb[3]
                k.mm(psr[:, 0:n], rrotT, kg[:, 0:n])
                t1 = t1r.next(); t2 = t2r.next()
                k.tt(t1[:, 0:n], kg[:, 0:n], cs[:, 0, t0 - LC:t0 - LC + n], ALU.mult)
                k.tt(t2[:, 0:n], psr[:, 0:n], cs[:, 1, t0 - LC:t0 - LC + n], ALU.mult)
                k.tt(t1[:, 0:n], t1[:, 0:n], t2[:, 0:n], ALU.add)
                k.tt(dst_fn(t0, n), t1[:, 0:n], rs[:, 0:n], ALU.mult)
            else:
                k.tt(dst_fn(t0, n), kg[:, 0:n], rs[:, 0:n], ALU.mult)

        import os
        upto = int(os.environ.get("ATT_UPTO", "9"))
        if upto < 2:
            return
        self._atp = 0
        qn = self.pvv("qn%d" % l)
        kn = self.pvv("kn%d" % l)
        jobs = []
        for (t0, n, isctx) in self.tblocks(True):
            jobs.append((256, kn, t0, n, (lambda a, b: KT[:, a:a + b]), not isctx))
            if (not isctx) or with_ctx_out:
                for c in range(2):
                    jobs.append((c * 128, qn, t0, n, (lambda a, b, c=c: QT[:, c, a:a + b]), not isctx))
        st = qk_stage1(*jobs[0][0:4])
        for i, jb in enumerate(jobs):
            nst = qk_stage1(*jobs[i + 1][0:4]) if i + 1 < len(jobs) else None
            qk_stage2(st, jb[4], jb[2], jb[3], jb[5])
            st = nst
        if upto < 3:
            return
        for j in range(NTILE):
            ps = self.psb[j % 2]
            for kk in range(8):
                k.mm(ps[:, 0:128], self.hT[:, kk, j * 128:(j + 1) * 128], wg[:, kk, 384:512], start=(kk == 0), stop=(kk == 7))
            vmode = os.environ.get("VMODE", "act")
            for kv in range(2):
                for dup in range(2):
                    if vmode == "none":
                        continue
                    k.copy(Vd[:, j, kv, dup * 64:(dup + 1) * 64], ps[:, kv * 64:(kv + 1) * 64],
                           eng=(("act" if dup else "dve") if vmode == "mix" else vmode))
        if upto < 4:
            return
        qblocks = self.tblocks(with_ctx_out)
        it = 0
        for (t0, n, isctx) in qblocks:
            tiles = [0, 1] if isctx else list(range(NTILE))
            mx = mxr.next()
            for c in range(2):
                for hp in range(2):
                    psO = self.psb[4 + it % 2]
                    psDn = self.psb[6 + it % 2]
                    it += 1
                    rows = slice(hp * 64, (hp + 1) * 64)
                    def s_stage(ji):
                        j = tiles[ji]
                        psS = self.psb[ji % 4]
                        k.mm(psS[:, 0:n], KT[rows, j * 128:(j + 1) * 128], QT[rows, c, t0:t0 + n])
                        pt = ptr.next()
                        k.act(pt[:, 0:n], psS[:, 0:n], AF.Exp, scale=0.125)
                        return pt

                    def pv_stage(ji, pt):
                        j = tiles[ji]
                        k.mm(psO[:, 0:n], Vd[:, j, hp, :], pt[:, 0:n], start=(ji == 0), stop=(ji == len(tiles) - 1))
                        k.mm(psDn[:, 0:n], self.onesb[:], pt[:, 0:n], start=(ji == 0), stop=(ji == len(tiles) - 1))

                    pts = {0: s_stage(0)}
                    for ji in range(len(tiles)):
                        if ji + 1 < len(tiles):
                            pts[ji + 1] = s_stage(ji + 1)
                        pv_stage(ji, pts.pop(ji))
                    rd = rdr.next()
                    k.recip(rd[rows, 0:n], psDn[rows, 0:n])
                    k.tt(mx[rows, c, 0:n], psO[rows, 0:n], rd[rows, 0:n], ALU.mult)
            if upto < 5:
                continue
            self.wout_apply(s, l, wo, 2, 128, mx, t0, n, isctx)

    def moe(self, s, l, with_ctx_out):
        P, k = self.P, self.k
        blocks = self.tblocks(with_ctx_out)
        tiles = list(range(0 if with_ctx_out else 2, NTILE))
        nt = NTILE
        o = OFF_SCR
        wr = P.sb("mo_wr", [128, 8, 20], BF16, o, group="scr"); o = wr.end
        lg = P.sb("mo_lg", [128, nt, 20], F32, o, group="scr"); o = lg.end
        comb = P.sb("mo_comb", [128, nt, 16], F32, o, group="scr"); o = comb.end
        sm = {}
        for nm, w in (("gmax", 1), ("gsh", 4), ("gsum", 1), ("gp", 1), ("ohg", 4), ("prod", 16), ("ing", 4), ("m1", 1),
                      ("oh1", 4), ("ing2", 4), ("m2", 1), ("oh2", 4), ("dl", 1), ("ex", 1), ("den", 1), ("p1", 1),
                      ("w1", 1), ("w2", 1), ("cw", 4), ("cw2", 4)):
            sm[nm] = P.sb("mo_" + nm, [128, nt, w], F32, o, group="scr"); o = sm[nm].end
        wsets = []
        for i in range(2):
            g = P.sb("mo_g%d" % i, [128, 8, 512], BF16, o, group="scr"); o = g.end
            u = P.sb("mo_u%d" % i, [128, 8, 512], BF16, o, group="scr"); o = u.end
            d = P.sb("mo_d%d" % i, [128, 4, 1024], BF16, o, group="scr"); o = d.end
            wsets.append((g, u, d))
        wrot = Rot(wsets)
        bcs = Rot([P.sb("mo_bc%d" % i, [128, 512], F32, o + i * 2048, group="scr") for i in range(2)]); o += 4096
        sgr = Rot([P.sb("mo_sg%d" % i, [128, 512], F32, o + i * 2048, group="scr") for i in range(2)]); o += 4096
        tr = Rot([P.sb("mo_t%d" % i, [128, 512], F32, o + i * 2048, group="scr") for i in range(2)]); o += 4096
        hwr = Rot([P.sb("mo_hw%d" % i, [128, 4, 512], BF16, o + i * 4096, group="scr") for i in range(2)]); o += 8192
        assert o <= SB_END, o
        self.load_w(wr[:], self.rw_d[l])
        def load_expert(e):
            g, u, d = wrot.next()
            self.load_w(g[:], self.wg_d[l, e].rearrange("(k p) n -> p k n", p=128))
            self.load_w(u[:], self.wu_d[l, e].rearrange("(k p) n -> p k n", p=128))
            self.load_w(d[:], self.wd_d[l, e].rearrange("(k p) n -> p k n", p=128))
            return g, u, d
        nxt = load_expert(0)
        ps = self.psrot.next()
        for j in tiles:
            for kk in range(8):
                k.mm(ps[:, j * 20:(j + 1) * 20], self.hT[:, kk, j * 128:(j + 1) * 128], wr[:, kk, :],
                     start=(kk == 0), stop=(kk == 7))
        j0, j1 = tiles[0], tiles[-1] + 1
        njt = j1 - j0
        rb = self.pvv("rb%d" % l)
        psv = ps[:, j0 * 20:j1 * 20]
        k.tt(lg[:, j0:j1, :], psv.w(psv.ap.rearrange("p (j c) -> p j c", c=20)),
             rb.w(rb.ap.unsqueeze(1).broadcast_to([128, njt, 20])), ALU.add)
        S = lambda nm: sm[nm][:, j0:j1, :]
        def bc(v, w):
            return v.w(v.ap.broadcast_to([128, njt, w]))
        gl = lg[:, j0:j1, 0:4]
        k.reduce(S("gmax"), gl, ALU.max)
        k.tt(S("gsh"), gl, bc(S("gmax"), 4), ALU.subtract)
        k.tt(S("ohg"), gl, bc(S("gmax"), 4), ALU.is_equal)
        k.act(S("gsh"), S("gsh"), AF.Exp)
        k.reduce(S("gsum"), S("gsh"), ALU.add)
        k.recip(S("gp"), S("gsum"))
        el = lg[:, j0:j1, 4:20]
        el4 = el.w(el.ap.rearrange("p j (g e) -> p j g e", e=4))
        ohg = S("ohg")
        pr = S("prod")
        pr4 = pr.w(pr.ap.rearrange("p j (g e) -> p j g e", e=4))
        k.tt(pr4, el4, ohg.w(ohg.ap.unsqueeze(3).broadcast_to([128, njt, 4, 4])), ALU.mult)
        k.reduce(S("ing"), pr.w(pr.ap.rearrange("p j (g e) -> p j e g", e=4)), ALU.add)
        k.reduce(S("m1"), S("ing"), ALU.max)
        k.tt(S("oh1"), S("ing"), bc(S("m1"), 4), ALU.is_equal)
        k.stt(S("ing2"), S("oh1"), -1e30, S("ing"), ALU.mult, ALU.add)
        k.reduce(S("m2"), S("ing2"), ALU.max)
        k.tt(S("oh2"), S("ing2"), bc(S("m2"), 4), ALU.is_equal)
        k.tt(S("dl"), S("m2"), S("m1"), ALU.subtract)
        k.act(S("ex"), S("dl"), AF.Exp)
        k.ts(S("den"), S("ex"), 1.0, ALU.add)
        k.recip(S("p1"), S("den"))
        k.tt(S("w1"), S("p1"), S("gp"), ALU.mult)
        k.tt(S("w2"), S("w1"), S("ex"), ALU.mult)
        k.tt(S("cw"), S("oh1"), bc(S("w1"), 4), ALU.mult)
        k.tt(S("cw2"), S("oh2"), bc(S("w2"), 4), ALU.mult)
        k.tt(S("cw"), S("cw"), S("cw2"), ALU.add)
        cb = comb[:, j0:j1, :]
        cw = S("cw")
        k.tt(cb.w(cb.ap.rearrange("p j (g e) -> p j g e", e=4)),
             ohg.w(ohg.ap.unsqueeze(3).broadcast_to([128, njt, 4, 4])),
             cw.w(cw.ap.unsqueeze(2).broadcast_to([128, njt, 4, 4])), ALU.mult)
        if self.debug and self.debug[0] == "comb" and s == 0 and l == 0:
            self.toks_out.append(k.store(self.dbg_d, comb[:]))
        ident = self.cv("ident")
        wmap = {0: nxt}
        if 16 > 1:
            wmap[1] = load_expert(1)
        items = [(e, bi) for e in range(16) for bi in range(len(blocks))]
        hws = {}

        def stage_a(e, bi):
            g, u, d = wmap[e]
            t0, n, isctx = blocks[bi]
            psB = self.psrot.next()
            for jj in range(n // 128):
                j = t0 // 128 + jj
                cv_ = comb[:, j, e:e + 1]
                k.mm(psB[:, jj * 128:(jj + 1) * 128], cv_.w(cv_.ap.broadcast_to([128, 128])), ident)
            bcb = bcs.next()
            k.copy(bcb[:, 0:n], psB[:, 0:n], eng="act")
            hw = hwr.next()
            hws[(e, bi)] = hw
            for fc in range(4):
                psG = self.psrot.next()
                self.proj_fm(psG[:, 0:n], g, fc * 128, 128, t0, n)
                psU = self.psrot.next()
                self.proj_fm(psU[:, 0:n], u, fc * 128, 128, t0, n)
                sg = sgr.next()
                k.act(sg[:, 0:n], psG[:, 0:n], AF.Silu)
                tt_ = tr.next()
                k.tt(tt_[:, 0:n], sg[:, 0:n], psU[:, 0:n], ALU.mult)
                k.tt(hw[:, fc, 0:n], tt_[:, 0:n], bcb[:, 0:n], ALU.mult)

        def stage_b(e, bi):
            g, u, d = wmap[e]
            t0, n, isctx = blocks[bi]
            col = 2 if isctx else s
            hw = hws.pop((e, bi))
            for oc in range(8):
                psD = self.psrot.next()
                for fc in range(4):
                    k.mm(psD[:, 0:n], d[:, fc, oc * 128:(oc + 1) * 128], hw[:, fc, 0:n], start=(fc == 0), stop=(fc == 3))
                self.resid_add(oc, t0, n, psD, self.mod[l][:, 40 + oc, col:col + 1])
            if bi == len(blocks) - 1 and e + 2 < 16:
                wmap[e + 2] = load_expert(e + 2)

        for i, it_ in enumerate(items):
            stage_a(*it_)
            if i > 0:
                stage_b(*items[i - 1])
        stage_b(*items[-1])


def pack_rw(inp):
    rw = np.concatenate([inp["router_group_w"], inp["router_expert_w"]], axis=2)
    return np.ascontiguousarray(rw.reshape(DEPTH, 8, 128, 20).transpose(0, 2, 1, 3), dtype=np.float32)


def pack_rl(inp):
    return np.ascontiguousarray(np.repeat(inp["ret_decay_logit"], 64, axis=-1), dtype=np.float32)


_NC_CACHE = {}


def kernel(**inp):
    inp = {k: np.asarray(v) for k, v in inp.items()}
    ncores = 8
    nseq = 2
    if "nc" not in _NC_CACHE:
        mk = MK(nseq=nseq, nlayers=DEPTH, stages=("hgrn", "conv", "ret", "attn", "moe"))
        _NC_CACHE["nc"] = mk.build()
        _NC_CACHE["mk"] = mk
    nc, mk = _NC_CACHE["nc"], _NC_CACHE["mk"]
    pv = pack_pv(inp, nseq)
    cst = mk.cstl.array()
    rw = pack_rw(inp)
    rope = rope_tables()
    hl = np.ascontiguousarray(inp["hgrn_lb_logits"], dtype=np.float32)
    rl = pack_rl(inp)
    maps = []
    for c in range(ncores):
        b0 = c * nseq
        cc = np.stack([fm(inp["c"][b0]), fm(inp["c"][b0 + 1]), fm(inp["c_ctx"])], axis=2).reshape(128, 24)
        maps.append({
            "x": np.ascontiguousarray(inp["x"][b0:b0 + nseq], dtype=np.float32),
            "ctx": np.ascontiguousarray(inp["ctx"][b0:b0 + nseq], dtype=np.float32),
            "cc": np.ascontiguousarray(cc, dtype=np.float32),
            "pv": pv, "cst": cst, "rope": rope, "hl": hl, "rl": rl, "rw": rw,
            "ada_w": inp["ada_w"], "w_in": inp["w_in"], "w_out": inp["w_out"],
            "wg": inp["expert_w_gate"], "wu": inp["expert_w_up"], "wd": inp["expert_w_down"],
        })
    res = run_bass_kernel_spmd(nc, maps, core_ids=list(range(ncores)))
    out = np.concatenate([np.asarray(r["out"]) for r in res.results], axis=0)
    return out.astype(np.float32)
```
